# Optimizing a Trainium2 kernel written in Bass

```python
import math
import jax, jax.numpy as jnp
from jax import lax
import numpy as np

D_MODEL = 1024
BATCH = 32
SEQ = 2048
DEPTH = 4
DEC_BATCH = 2
DEC_SEQ = 8192
PAST_LEN = 128

HEAD_DIM = 64
DIFF_HEADS = D_MODEL // (4 * HEAD_DIM)
DIFF_VDIM = 2 * HEAD_DIM
DIFF_WIDTH = DIFF_HEADS * DIFF_VDIM
SWA_HEADS = D_MODEL // (2 * HEAD_DIM)
SWA_KV_HEADS = SWA_HEADS // 4
SWA_GROUP = SWA_HEADS // SWA_KV_HEADS
SWA_WIDTH = SWA_HEADS * HEAD_DIM
MIX_WIDTH = DIFF_WIDTH + SWA_WIDTH
WINDOW = 128
BLOCK = 128
QBLOCK = 128
N_EXPERTS = 16
EXPERT_FF = 1024
CAPACITY_FACTOR = 2
ROPE_THETA = 10000.0
EPS = 1e-6
W_DQ = DIFF_HEADS * 2 * HEAD_DIM
W_DK = DIFF_HEADS * 2 * HEAD_DIM
W_DV = DIFF_WIDTH
W_SQ = SWA_HEADS * HEAD_DIM
W_SK = SWA_KV_HEADS * HEAD_DIM
W_SV = SWA_KV_HEADS * HEAD_DIM
IN_WIDTH = W_DQ + W_DK + W_DV + W_SQ + W_SK + W_SV

kernel_name = "hymba_diff_band_gqa_expert_choice_encoder"


def rmsnorm(x, g):
    xf = x.astype(jnp.float32)
    y = xf * lax.rsqrt(jnp.mean(xf * xf, axis=-1, keepdims=True) + EPS) * g.astype(jnp.float32)
    return y.astype(x.dtype)


def rope(x):
    S, d = x.shape[1], x.shape[-1]
    inv = 1.0 / (ROPE_THETA ** (jnp.arange(0, d, 2, dtype=jnp.float32) / d))
    ang = jnp.arange(S, dtype=jnp.float32)[:, None] * inv[None, :]
    ang = jnp.concatenate([ang, ang], axis=-1)
    shape = (1, S) + (1,) * (x.ndim - 3) + (d,)
    cos = jnp.cos(ang).reshape(shape)
    sin = jnp.sin(ang).reshape(shape)
    xf = x.astype(jnp.float32)
    x1, x2 = xf[..., : d // 2], xf[..., d // 2:]
    rot = jnp.concatenate([-x2, x1], axis=-1)
    return (xf * cos + rot * sin).astype(x.dtype)


def diff_attention(q, k, v, lam):
    B, S, H, _, d = q.shape
    nb = S // QBLOCK
    scale = 1.0 / math.sqrt(d)
    qb = q.reshape(B, nb, QBLOCK, H, 2, d).transpose(1, 0, 2, 3, 4, 5)

    def one_block(qblk):
        s = jnp.einsum('bqhcd,bkhcd->bhcqk', qblk, k).astype(jnp.float32) * scale
        p = jax.nn.softmax(s, axis=-1)
        a = p[:, :, 0] - lam * p[:, :, 1]
        return jnp.einsum('bhqk,bkhe->bqhe', a.astype(v.dtype), v)

    o = lax.map(one_block, qb)
    return o.transpose(1, 0, 2, 3, 4).reshape(B, S, H, v.shape[-1])


def band_gqa_attention(q, k, v, sink):
    B, S, Hkv, G, d = q.shape
    nb = S // BLOCK
    scale = 1.0 / math.sqrt(d)
    pad = ((0, 0), (BLOCK, BLOCK), (0, 0), (0, 0))
    kp = jnp.pad(k, pad)
    vp = jnp.pad(v, pad)

    def bands(t):
        return jnp.concatenate(
            [t[:, i * BLOCK: i * BLOCK + S].reshape(B, nb, BLOCK, Hkv, d) for i in range(3)], axis=2)

    kb, vb = bands(kp), bands(vp)
    qb = q.reshape(B, nb, BLOCK, Hkv, G, d)
    s = jnp.einsum('bnqhgd,bnkhd->bnhgqk', qb, kb).astype(jnp.float32) * scale
    blk = jnp.arange(nb)[:, None, None] * BLOCK
    qpos = blk + jnp.arange(BLOCK)[None, :, None]
    kpos = blk - BLOCK + jnp.arange(3 * BLOCK)[None, None, :]
    valid = (jnp.abs(qpos - kpos) <= WINDOW) & (kpos >= 0) & (kpos < S)
    s = jnp.where(valid[None, :, None, None], s, -1e30)
    sink_col = jnp.broadcast_to(sink.astype(jnp.float32)[None, None, :, :, None, None],
                                s.shape[:-1] + (1,))
    p = jax.nn.softmax(jnp.concatenate([s, sink_col], axis=-1), axis=-1)[..., :-1]
    o = jnp.einsum('bnhgqk,bnkhd->bnqhgd', p.astype(v.dtype), vb)
    return o.reshape(B, S, Hkv * G * d)


def expert_choice_ffn(x, w_router, w_gate, w_up, w_down):
    B, S, D = x.shape
    T = B * S
    C = CAPACITY_FACTOR * T // N_EXPERTS
    xt = x.reshape(T, D)
    aff = jax.nn.softmax(jnp.dot(xt, w_router).astype(jnp.float32), axis=-1)
    gates, idx = lax.top_k(aff.T, C)
    xe = xt[idx]
    h = jax.nn.silu(jnp.einsum('ecd,edf->ecf', xe, w_gate)) * jnp.einsum('ecd,edf->ecf', xe, w_up)
    ye = jnp.einsum('ecf,efd->ecd', h, w_down) * gates[..., None].astype(x.dtype)
    y = jnp.zeros((T, D), x.dtype).at[idx.reshape(-1)].add(ye.reshape(-1, D))
    return y.reshape(B, S, D)


def trunk(x, attn_norm, w_in, diff_q_norm, diff_k_norm, lambda_q1, lambda_k1, lambda_q2, lambda_k2,
          diff_subln, swa_q_norm, swa_k_norm, swa_sink, w_out, ffn_norm, w_router, w_gate, w_up, w_down):
    B, S, _ = x.shape
    offs = np.cumsum([W_DQ, W_DK, W_DV, W_SQ, W_SK]).tolist()
    for l in range(DEPTH):
        lam_init = 0.8 - 0.6 * math.exp(-0.3 * l)
        z = jnp.dot(rmsnorm(x, attn_norm[l]), w_in[l])
        dq, dk, dv, sq, sk, sv = jnp.split(z, offs, axis=-1)
        dq = rope(rmsnorm(dq.reshape(B, S, DIFF_HEADS, 2, HEAD_DIM), diff_q_norm[l]))
        dk = rope(rmsnorm(dk.reshape(B, S, DIFF_HEADS, 2, HEAD_DIM), diff_k_norm[l]))
        dv = dv.reshape(B, S, DIFF_HEADS, DIFF_VDIM)
        lam = (jnp.exp(jnp.sum(lambda_q1[l].astype(jnp.float32) * lambda_k1[l].astype(jnp.float32)))
               - jnp.exp(jnp.sum(lambda_q2[l].astype(jnp.float32) * lambda_k2[l].astype(jnp.float32)))
               + lam_init)
        do = diff_attention(dq, dk, dv, lam)
        do = (rmsnorm(do, diff_subln[l]) * (1.0 - lam_init)).reshape(B, S, DIFF_WIDTH)
        sq = rope(rmsnorm(sq.reshape(B, S, SWA_KV_HEADS, SWA_GROUP, HEAD_DIM), swa_q_norm[l]))
        sk = rope(rmsnorm(sk.reshape(B, S, SWA_KV_HEADS, HEAD_DIM), swa_k_norm[l]))
        sv = sv.reshape(B, S, SWA_KV_HEADS, HEAD_DIM)
        so = band_gqa_attention(sq, sk, sv, swa_sink[l].reshape(SWA_KV_HEADS, SWA_GROUP))
        x = x + jnp.dot(jnp.concatenate([do, so], axis=-1), w_out[l])
        x = x + expert_choice_ffn(rmsnorm(x, ffn_norm[l]), w_router[l], w_gate[l], w_up[l], w_down[l])
    return x


def setup_inputs(seed: int = 0) -> dict:
    key = jax.random.key(seed)
    ks = jax.random.split(key, 24)
    f32 = jnp.float32
    nrm = lambda k, shape, s: jax.random.normal(k, shape, f32) * s
    gain = lambda k, shape: 1.0 + 0.05 * jax.random.normal(k, shape, f32)
    return {
        "x_prompt": nrm(ks[0], (BATCH, SEQ, D_MODEL), 1.0),
        "x_sample": nrm(ks[1], (DEC_BATCH, DEC_SEQ, D_MODEL), 1.0),
        "attn_norm": gain(ks[2], (DEPTH, D_MODEL)),
        "w_in": nrm(ks[3], (DEPTH, D_MODEL, IN_WIDTH), D_MODEL ** -0.5),
        "diff_q_norm": gain(ks[4], (DEPTH, HEAD_DIM)),
        "diff_k_norm": gain(ks[5], (DEPTH, HEAD_DIM)),
        "lambda_q1": nrm(ks[6], (DEPTH, HEAD_DIM), 0.1),
        "lambda_k1": nrm(ks[7], (DEPTH, HEAD_DIM), 0.1),
        "lambda_q2": nrm(ks[8], (DEPTH, HEAD_DIM), 0.1),
        "lambda_k2": nrm(ks[9], (DEPTH, HEAD_DIM), 0.1),
        "diff_subln": gain(ks[10], (DEPTH, DIFF_VDIM)),
        "swa_q_norm": gain(ks[11], (DEPTH, HEAD_DIM)),
        "swa_k_norm": gain(ks[12], (DEPTH, HEAD_DIM)),
        "swa_sink": nrm(ks[13], (DEPTH, SWA_HEADS), 0.5),
        "w_out": nrm(ks[14], (DEPTH, MIX_WIDTH, D_MODEL), MIX_WIDTH ** -0.5),
        "ffn_norm": gain(ks[15], (DEPTH, D_MODEL)),
        "w_router": nrm(ks[16], (DEPTH, D_MODEL, N_EXPERTS), D_MODEL ** -0.5),
        "w_gate": nrm(ks[17], (DEPTH, N_EXPERTS, D_MODEL, EXPERT_FF), D_MODEL ** -0.5),
        "w_up": nrm(ks[18], (DEPTH, N_EXPERTS, D_MODEL, EXPERT_FF), D_MODEL ** -0.5),
        "w_down": nrm(ks[19], (DEPTH, N_EXPERTS, EXPERT_FF, D_MODEL), EXPERT_FF ** -0.5),
    }


def reference(x_prompt, x_sample, attn_norm, w_in, diff_q_norm, diff_k_norm, lambda_q1, lambda_k1,
              lambda_q2, lambda_k2, diff_subln, swa_q_norm, swa_k_norm, swa_sink, w_out, ffn_norm,
              w_router, w_gate, w_up, w_down):
    y_prompt = trunk(x_prompt, attn_norm, w_in, diff_q_norm, diff_k_norm, lambda_q1, lambda_k1,
                     lambda_q2, lambda_k2, diff_subln, swa_q_norm, swa_k_norm, swa_sink, w_out,
                     ffn_norm, w_router, w_gate, w_up, w_down)
    y_sample = trunk(x_sample, attn_norm, w_in, diff_q_norm, diff_k_norm, lambda_q1, lambda_k1,
                     lambda_q2, lambda_k2, diff_subln, swa_q_norm, swa_k_norm, swa_sink, w_out,
                     ffn_norm, w_router, w_gate, w_up, w_down)
    return (y_prompt, y_sample)
```

```python
import math
import numpy as np
from concourse.bass_utils import run_bass_kernel_spmd
import contextlib
import concourse.bass as bass
import concourse.mybir as mybir

F32 = mybir.dt.float32
BF16 = mybir.dt.bfloat16
I32 = mybir.dt.int32
AF = mybir.ActivationFunctionType
ALU = mybir.AluOpType
AX = mybir.AxisListType


class Res:
    __slots__ = ("name", "w", "r", "ld", "st", "depth")

    def __init__(self, name):
        self.name = name
        self.w = {}
        self.r = {}
        self.ld = None
        self.st = None
        self.depth = 0


class Sched:
    ENG = ("pe", "act", "dve", "pool", "sp")

    def __init__(self, nc, stack):
        self.nc = nc
        self.stack = stack
        self.root = stack
        self.scopes = []
        self.free_sems = []
        self.q = {e: [] for e in self.ENG}
        self.semh = {}
        self.semc = {}
        self.known = {e: {} for e in self.ENG}
        self.esem = {}
        for e in ("pe", "act", "dve", "pool"):
            self.esem[e] = self.newsem("E_" + e)
        self.nsem = 0
        self.uid = 0
        self.ckeys = {}
        self.ninstr = 0

    def newsem(self, key, persistent=False):
        if self.free_sems and not key.startswith("E_"):
            k = self.free_sems.pop()
        else:
            k = "s%d_%s" % (len(self.semh), key[:20])
            self.semh[k] = self.root.enter_context(self.nc.semaphore(k))
            self.semc[k] = 0
        if self.scopes and not persistent:
            self.scopes[-1][1].append(k)
        return k

    @contextlib.contextmanager
    def scope(self):
        self.barrier()
        st = contextlib.ExitStack()
        self.scopes.append((st, []))
        old = self.stack
        self.stack = st
        try:
            yield
        finally:
            self.barrier()
            self.stack = old
            _, sems = self.scopes.pop()
            self.free_sems.extend(sems)
            st.close()

    def sb(self, name, shape, dt):
        self.uid += 1
        name = "%s_%d" % (name, self.uid)
        t = self.stack.enter_context(self.nc.sbuf_tensor(name, list(shape), dt))
        r = Res(name)
        r.depth = len(self.scopes)
        return t, r

    def ps(self, name, shape, dt=F32):
        self.uid += 1
        name = "%s_%d" % (name, self.uid)
        t = self.stack.enter_context(self.nc.psum_tensor(name, list(shape), dt))
        r = Res(name)
        r.depth = len(self.scopes)
        return t, r

    def _collect(self, eng, reads, writes):
        waits = {}

        def need(d):
            for k, v in d.items():
                if waits.get(k, 0) < v:
                    waits[k] = v
        own = self.esem.get(eng)
        for r in reads:
            need(r.w)
        for w in writes:
            need(w.w)
            for k, v in w.r.items():
                if k == own:
                    continue
                if waits.get(k, 0) < v:
                    waits[k] = v
        kn = self.known[eng]
        wl = []
        for k, v in waits.items():
            if eng == "pe" and k == own:
                continue
            if kn.get(k, 0) >= v:
                continue
            kn[k] = v
            wl.append((k, v))
        return wl

    def op(self, eng, name, kw, reads=(), writes=()):
        fn = (name, kw)
        wl = self._collect(eng, reads, writes)
        k = self.esem[eng]
        self.semc[k] += 1
        c = self.semc[k]
        for r in reads:
            if r.r.get(k, 0) < c:
                r.r[k] = c
        for w in writes:
            w.w = {k: c}
            w.r = {}
        self.q[eng].append((wl, fn, k, 1))
        self.ninstr += 1

    def dma(self, qeng, kw, sb_res, direction, reads=(), writes=(), merge=False):
        fn = ("dma_start", kw)
        if direction == "ld":
            if sb_res.ld is None:
                sb_res.ld = self.newsem("L_" + sb_res.name, persistent=(sb_res.depth == 0))
            key = sb_res.ld
        else:
            if sb_res.st is None:
                sb_res.st = self.newsem("S_" + sb_res.name, persistent=(sb_res.depth == 0))
            key = sb_res.st
        saved = []
        for w in writes:
            if merge:
                saved.append(dict(w.w))
                w.w = {}
            elif key in w.w:
                w.w.pop(key)
        wl = self._collect(qeng, reads, writes)
        self.semc[key] += 16
        c = self.semc[key]
        for r in reads:
            if r.r.get(key, 0) < c:
                r.r[key] = c
        for i, w in enumerate(writes):
            if merge:
                old = saved[i]
                old[key] = c
                w.w = old
            else:
                w.w = {key: c}
                w.r = {}
        self.q[qeng].append((wl, fn, key, 16))
        self.ninstr += 1

    def custom(self, eng, name, kw, key, inc, reads=(), writes=()):
        fn = (name, kw)
        if key not in self.ckeys:
            self.ckeys[key] = self.newsem(key, persistent=True)
        key = self.ckeys[key]
        wl = self._collect(eng, reads, writes)
        self.semc[key] += inc
        c = self.semc[key]
        for r in reads:
            if r.r.get(key, 0) < c:
                r.r[key] = c
        for w in writes:
            w.w = {key: c}
            w.r = {}
        self.q[eng].append((wl, fn, key, inc))
        self.ninstr += 1

    def barrier(self):
        allv = {k: v for k, v in self.semc.items() if v > 0}
        for e in self.ENG:
            kn = self.known[e]
            wl = []
            for k, v in allv.items():
                if kn.get(k, 0) >= v:
                    continue
                kn[k] = v
                wl.append((k, v))
            if wl:
                self.q[e].append((wl, None, None, 0))

    def emit(self):
        nc = self.nc
        engs = {"pe": "tensor", "act": "scalar", "dve": "vector", "pool": "gpsimd", "sp": "sync"}
        with nc.Block() as block:
            for e in self.ENG:
                items = self.q[e]
                if not items:
                    continue

                def body(eng, items=items):
                    for wl, fn, key, inc in items:
                        for k, v in wl:
                            eng.wait_ge(self.semh[k], v)
                        if fn is not None:
                            ins = getattr(eng, fn[0])(**fn[1])
                            ins.then_inc(self.semh[key], inc)
                getattr(block, engs[e])(body)


def bc(ap_tensor, offset, dims):
    return bass.AP(ap_tensor, offset, [list(d) for d in dims])
D = 1024
NE = 16
INW = 2304
EPS = 1e-6
NITER = 24


def AP(t, off, dims):
    return bass.AP(t, off, [list(d) for d in dims])


def V(a, dims):
    return bass.AP(a.tensor, a.offset, [list(a.ap[0])] + [list(d) for d in dims])


class StopBuild(Exception):
    pass


def build(cfg):
    L, NPS, SEG = cfg["L"], cfg["NPS"], cfg["SEG"]
    KC = SEG // 128
    TPT = NPS * KC
    TST = KC
    TT = TPT + TST
    NTOK = TT * 128
    NU = NPS + 1
    NB = TT // 4
    CAPT = cfg.get("CAPT", 2)
    G = max(1, 4 // CAPT)
    while NB % G:
        G //= 2
    NS = G * CAPT * 128
    BT = []
    if NPS % 4 == 0 and cfg.get("mix", True):
        for q in range(NPS // 4):
            for j in range(KC):
                BT.append([(4 * q + s_) * KC + j for s_ in range(4)])
    else:
        for b in range(TPT // 4):
            BT.append([4 * b + t for t in range(4)])
    for b in range(TST // 4):
        BT.append([TPT + 4 * b + t for t in range(4)])
    assert len(BT) == NB
    QG = SEG // 512
    CW = 12 * SEG
    CAPS = [2 * (8 * TPT * 128) // NE, 2 * (8 * TST * 128) // NE]
    oV = 4 * SEG
    oSK = oV + KC * 512
    oSV = oSK + 2 * SEG
    NSUB = 2

    nc = bass.Bass("TRN2", target_bir_lowering=False)
    dt = nc.dram_tensor
    xp = dt("xp", [TPT * 128, D], F32, kind="ExternalInput")
    xs = dt("xs", [TST * 128, D], F32, kind="ExternalInput")
    ropet = dt("ropet", [NTOK, 128], F32, kind="ExternalInput")
    cst = dt("cst", [128, 640], F32, kind="ExternalInput")
    cct = dt("cct", [128, 16], F32, kind="ExternalInput")
    attn_norm = dt("attn_norm", [L, D], F32, kind="ExternalInput")
    w_in = dt("w_in", [L, D, INW], F32, kind="ExternalInput")
    g4 = dt("g4", [L, 256], F32, kind="ExternalInput")
    lam4 = dt("lam4", [L, 256], F32, kind="ExternalInput")
    subln = dt("subln", [L, 128], F32, kind="ExternalInput")
    sink = dt("sink", [L, 8], F32, kind="ExternalInput")
    w_out = dt("w_out", [L, D, D], F32, kind="ExternalInput")
    ffn_norm = dt("ffn_norm", [L, D], F32, kind="ExternalInput")
    w_router = dt("w_router", [L, D, NE], F32, kind="ExternalInput")
    w_gate = dt("w_gate", [L, NE, D, D], F32, kind="ExternalInput")
    w_up = dt("w_up", [L, NE, D, D], F32, kind="ExternalInput")
    w_down = dt("w_down", [L, NE, D, D], F32, kind="ExternalInput")
    yp = dt("yp", [TPT * 128, D], F32, kind="ExternalOutput")
    ys = dt("ys", [TST * 128, D], F32, kind="ExternalOutput")
    QT_d = dt("QT_d", [4, 128, NTOK], BF16, kind="Internal")
    KT_d = dt("KT_d", [4, 128, TPT * 128], BF16, kind="Internal")
    V_d = dt("V_d", [TPT * 128, 512], BF16, kind="Internal")
    SQT_d = dt("SQT_d", [8, 64, NTOK], BF16, kind="Internal")
    SKT_d = dt("SKT_d", [2, 64, TPT * 128], BF16, kind="Internal")
    SV_d = dt("SV_d", [TPT * 128, 256], BF16, kind="Internal")
    KTs_in = dt("KTs_in", [4, 128, SEG], BF16, kind="Internal")
    KTs_g = dt("KTs_g", [4, 512, SEG], BF16, kind="Internal")
    Vs_in = dt("Vs_in", [4, 128, KC * 128], BF16, kind="Internal")
    Vs_g = dt("Vs_g", [4, 512, KC * 128], BF16, kind="Internal")
    SKs_in = dt("SKs_in", [2, 64, SEG], BF16, kind="Internal")
    SKs_g = dt("SKs_g", [2, 256, SEG], BF16, kind="Internal")
    SVs_in = dt("SVs_in", [2, 128, KC * 128], BF16, kind="Internal")
    SVs_g = dt("SVs_g", [2, 512, KC * 128], BF16, kind="Internal")
    XN_d = dt("XN_d", [NTOK, D], BF16, kind="Internal")
    H_d = dt("H_d", [NTOK, D], F32, kind="Internal")
    X_d = dt("X_d", [NTOK, D], F32, kind="Internal")
    AFF_d = dt("AFF_d", [128, TT * NE], F32, kind="Internal")
    AFFG_d = dt("AFFG_d", [8 * 128, TT * NE], F32, kind="Internal")
    YE_d = dt("YE_d", [NB, NE, CAPT, 128, D], BF16, kind="Internal")

    rs = {n: Res(n) for n in ["xin", "rope", "cst", "cct", "wts", "QT_d", "KT_d", "V_d", "SQT_d", "SKT_d",
                              "SV_d", "SKV_in", "SKV_g", "XN_d", "H_d", "X_d", "AFF_d", "AFFG_d", "YE_d", "yout"]}

    def xsrc_ap(l, tile):
        if l == 0:
            return (xp.ap()[tile * 128:(tile + 1) * 128, :] if tile < TPT
                    else xs.ap()[(tile - TPT) * 128:(tile - TPT + 1) * 128, :]), rs["xin"]
        return X_d.ap()[tile * 128:(tile + 1) * 128, :], rs["X_d"]

    def xdst_ap(l, tile):
        if l == L - 1:
            return (yp.ap()[tile * 128:(tile + 1) * 128, :] if tile < TPT
                    else ys.ap()[(tile - TPT) * 128:(tile - TPT + 1) * 128, :]), rs["yout"]
        return X_d.ap()[tile * 128:(tile + 1) * 128, :], rs["X_d"]

    def bcast_rows(a):
        return AP(a.tensor, a.offset, [[0, 128]] + [list(x) for x in a.ap[1:]])

    with contextlib.ExitStack() as root:
        S = Sched(nc, root)
        op, dma = S.op, S.dma
        MUL, ADD, SUB = ALU.mult, ALU.add, ALU.subtract

        def mm(out, lhsT, rhs, start, stop, reads, writes):
            op("pe", "matmul", dict(out=out, lhsT=lhsT, rhs=rhs, start=start, stop=stop), reads, writes)

        def tr(out, in_, reads, writes):
            op("pe", "transpose", dict(out=out, in_=in_, identity=ident[:]), reads + [ident_r], writes)

        cstf, cstf_r = S.sb("cstf", [128, 640], F32)
        cc_t, cc_r = S.sb("cc_t", [128, 16], F32)
        ident, ident_r = S.sb("ident", [128, 128], BF16)
        Utri, Utri_r = S.sb("Utri", [128, 128], BF16)
        triL, triL_r = S.sb("triL", [128, 128], BF16)
        triR, triR_r = S.sb("triR", [128, 128], BF16)
        triLe, triLe_r = S.sb("triLe", [128, 128], BF16)
        triRe, triRe_r = S.sb("triRe", [128, 128], BF16)
        onesb, onesb_r = S.sb("onesb", [128, 128], BF16)
        onesf, onesf_r = S.sb("onesf", [128, 128], F32)
        AFFS, AFFS_r = S.sb("AFFS", [128, TT, NE], F32)
        posm, posm_r = S.sb("posm", [128, TT, NE], F32)
        g_attn, g_attn_r = S.sb("g_attn", [128, D], F32)
        g_ffn, g_ffn_r = S.sb("g_ffn", [128, D], F32)
        G4, G4_r = S.sb("G4", [128, 256], F32)
        L4, L4_r = S.sb("L4", [128, 256], F32)
        ltmp, ltmp_r = S.sb("ltmp", [128, 2, 64], F32)
        lsc, lsc_r = S.sb("lsc", [128, 8], F32)
        sublnS, sublnS_r = S.sb("sublnS", [128, 1], F32)
        lamneg, lamneg_r = S.sb("lamneg", [128, 1], F32)
        esink, esink_r = S.sb("esink", [128, 8], F32)
        wrf, wrf_r = S.sb("wrf", [128, 8, NE], F32)
        wrb, wrb_r = S.sb("wrb", [128, 8, NE], BF16)
        iota = cstf[:, 512:640]
        epsb, epsb_r = S.sb("epsb", [128, 1], F32)
        iota2, iota2_r = S.sb("iota2", [128, CAPT, 128], F32)
        op("dve", "memset", dict(ap=epsb[:], constant=EPS), [], [epsb_r])

        dma("sp", dict(out=cstf[:], in_=cst.ap()), cstf_r, "ld", [rs["cst"]], [cstf_r])
        dma("sp", dict(out=cc_t[:], in_=cct.ap()), cc_r, "ld", [rs["cct"]], [cc_r])
        for i, (t, r) in enumerate([(ident, ident_r), (Utri, Utri_r), (triL, triL_r), (triR, triR_r)]):
            op("dve", "tensor_copy", dict(out=t[:], in_=cstf[:, i * 128:(i + 1) * 128]), [cstf_r], [r])
        op("dve", "tensor_scalar", dict(out=triLe[:], in0=cstf[:, 256:384], scalar1=cc_t[:, 0:1], scalar2=None, op0=MUL), [cstf_r, cc_r], [triLe_r])
        op("dve", "tensor_scalar", dict(out=triRe[:], in0=cstf[:, 384:512], scalar1=cc_t[:, 1:2], scalar2=None, op0=MUL), [cstf_r, cc_r], [triRe_r])
        op("dve", "memset", dict(ap=onesb[:], constant=1.0), [], [onesb_r])
        op("dve", "memset", dict(ap=onesf[:], constant=1.0), [], [onesf_r])
        for s_ in range(CAPT):
            op("dve", "tensor_scalar", dict(out=iota2[:, s_, :], in0=iota, scalar1=float(128 * s_), scalar2=None, op0=ADD), [cstf_r], [iota2_r])

        for l in range(L):
          try:
              lam_init = 0.8 - 0.6 * math.exp(-0.3 * l)
              W = rs["wts"]
              dma("sp", dict(out=g_attn[:], in_=bcast_rows(attn_norm.ap()[l:l + 1, :])), g_attn_r, "ld", [W], [g_attn_r])
              dma("sp", dict(out=g_ffn[:], in_=bcast_rows(ffn_norm.ap()[l:l + 1, :])), g_ffn_r, "ld", [W], [g_ffn_r])
              dma("sp", dict(out=G4[:], in_=bcast_rows(g4.ap()[l:l + 1, :])), G4_r, "ld", [W], [G4_r])
              dma("sp", dict(out=L4[:], in_=bcast_rows(lam4.ap()[l:l + 1, :])), L4_r, "ld", [W], [L4_r])
              dma("sp", dict(out=esink[:], in_=bcast_rows(sink.ap()[l:l + 1, :])), esink_r, "ld", [W], [esink_r])
              dma("sp", dict(out=sublnS[:], in_=subln.ap()[l:l + 1, :].rearrange("o p -> p o")), sublnS_r, "ld", [W], [sublnS_r])
              dma("sp", dict(out=wrf[:], in_=w_router.ap()[l].rearrange("(c p) e -> p c e", p=128)), wrf_r, "ld", [W], [wrf_r])
              op("dve", "tensor_copy", dict(out=wrb[:], in_=wrf[:]), [wrf_r], [wrb_r])
              op("dve", "tensor_scalar", dict(out=sublnS[:], in0=sublnS[:], scalar1=float(1.0 - lam_init), scalar2=None, op0=MUL), [sublnS_r], [sublnS_r])
              op("act", "activation", dict(out=esink[:], in_=esink[:], func=AF.Exp), [esink_r], [esink_r])
              op("dve", "tensor_tensor", dict(out=ltmp[:], in0=V(L4[:, 0:1], [[128, 2], [1, 64]]), in1=V(L4[:, 64:65], [[128, 2], [1, 64]]), op=MUL), [L4_r], [ltmp_r])
              op("dve", "tensor_reduce", dict(out=lsc[:, 0:2], in_=ltmp[:], axis=AX.X, op=ADD), [ltmp_r], [lsc_r])
              op("act", "activation", dict(out=lsc[:, 2:4], in_=lsc[:, 0:2], func=AF.Exp), [lsc_r], [lsc_r])
              op("dve", "tensor_tensor", dict(out=lsc[:, 4:5], in0=lsc[:, 3:4], in1=lsc[:, 2:3], op=SUB), [lsc_r], [lsc_r])
              op("dve", "tensor_scalar", dict(out=lamneg[:], in0=lsc[:, 4:5], scalar1=float(-lam_init), scalar2=None, op0=ADD), [lsc_r], [lamneg_r])

              if cfg.get("stop") == "S":
                  raise StopBuild()
              with S.scope():
                  Win, Win_r = S.sb("Win", [128, 8, INW], BF16)
                  stg = [S.sb("stg%d" % i, [128, INW], F32) for i in range(2)]
                  for c in range(8):
                      st_t, st_r = stg[c % 2]
                      dma("sp", dict(out=st_t[:], in_=w_in.ap()[l, c * 128:(c + 1) * 128, :]), st_r, "ld", [W], [st_r])
                      op("pool", "tensor_copy", dict(out=Win[:, c, :], in_=st_t[:]), [st_r], [Win_r])
                  xt = [S.sb("xt%d" % i, [128, D], F32) for i in range(2)]
                  rp = [S.sb("rp%d" % i, [128, 128], F32) for i in range(2)]
                  junk, junk_r = S.sb("junk", [128, D], F32)
                  ss1, ss1_r = S.sb("ss1", [128, 4], F32)
                  xn, xn_r = S.sb("xn", [128, D], BF16)
                  xnT, xnT_r = S.sb("xnT", [128, 8, 128], BF16)
                  zq, zq_r = S.sb("zq", [128, 26, 64], F32)
                  zsq, zsq_r = S.sb("zsq", [128, 26, 64], F32)
                  hss, hss_r = S.sb("hss", [128, 26], F32)
                  hrs, hrs_r = S.sb("hrs", [128, 26], F32)
                  zn, zn_r = S.sb("zn", [128, 26, 64], F32)
                  t1, t1_r = S.sb("t1", [128, 26, 64], F32)
                  t2, t2_r = S.sb("t2", [128, 26, 64], F32)
                  zr, zr_r = S.sb("zr", [128, 26 * 64], BF16)
                  vsb = [S.sb("vsb%d" % i, [128, 512], BF16) for i in range(2)]
                  svd = [S.sb("svd%d" % i, [128, 2, 2, 64], BF16) for i in range(2)]
                  qst = [S.sb("qst%d" % i, [128, 8, NSUB * 128], BF16) for i in range(2)]
                  sst = [S.sb("sst%d" % i, [64, 10, NSUB * 128], BF16) for i in range(2)]
                  zps, zps_r = S.ps("zps", [128, 2560], F32)
                  tpb, tpb_r = S.ps("tpb", [128, 8, 128], BF16)
                  tqb, tqb_r = S.ps("tqb", [128, 8, 128], BF16)
                  tsb, tsb_r = S.ps("tsb", [64, 8, 128], BF16)

                  order = list(range(TPT, TT)) + list(range(TPT))
                  for it, tile in enumerate(order[:cfg.get("atiles", 10 ** 9)]):
                      samp = tile >= TPT
                      lt = tile - TPT if samp else tile
                      x_t, x_r = xt[it % 2]
                      r_t, r_r = rp[it % 2]
                      src, src_r = xsrc_ap(l, tile)
                      dma("sp", dict(out=x_t[:], in_=src), x_r, "ld", [src_r], [x_r])
                      dma("sp", dict(out=r_t[:], in_=ropet.ap()[tile * 128:(tile + 1) * 128, :]), r_r, "ld", [rs["rope"]], [r_r])
                      op("act", "activation", dict(out=junk[:], in_=x_t[:], func=AF.Square, accum_out=ss1[:, 0:1]), [x_r], [junk_r, ss1_r])
                      op("act", "activation", dict(out=ss1[:, 1:2], in_=ss1[:, 0:1], func=AF.Sqrt, scale=1.0 / D, bias=epsb[:, 0:1]), [ss1_r, epsb_r], [ss1_r])
                      op("dve", "reciprocal", dict(out=ss1[:, 2:3], in_=ss1[:, 1:2]), [ss1_r], [ss1_r])
                      op("dve", "scalar_tensor_tensor", dict(out=xn[:], in0=x_t[:], scalar=ss1[:, 2:3], in1=g_attn[:], op0=MUL, op1=MUL), [x_r, ss1_r, g_attn_r], [xn_r])
                      for c in range(8):
                          tr(tpb[:, c, :], xn[:, c * 128:(c + 1) * 128], [xn_r], [tpb_r])
                      op("act", "activation", dict(out=xnT[:], in_=tpb[:], func=AF.Copy), [tpb_r], [xnT_r])
                      for ng in range(5):
                          n0, n1 = ng * 512, min(INW, ng * 512 + 512)
                          for c in range(8):
                              mm(zps[:, n0:n1], xnT[:, c, :], Win[:, c, n0:n1], c == 0, c == 7, [xnT_r, Win_r], [zps_r])
                      zqf = zq[:].rearrange("p a b -> p (a b)")
                      op("act", "activation", dict(out=zqf[:, 0:1024], in_=zps[:, 0:1024], func=AF.Copy), [zps_r], [zq_r])
                      op("act", "activation", dict(out=zqf[:, 1024:1664], in_=zps[:, 1536:2176], func=AF.Copy), [zps_r], [zq_r])
                      v_t, v_r = vsb[it % 2]
                      sv_t, sv_r = svd[it % 2]
                      op("act", "activation", dict(out=v_t[:], in_=zps[:, 1024:1536], func=AF.Copy), [zps_r], [v_r])
                      for dup in range(2):
                          op("act", "activation", dict(out=sv_t[:, :, dup, :], in_=zps[:, 2176:2304].rearrange("p (h d) -> p h d", d=64), func=AF.Copy), [zps_r], [sv_r])
                      op("dve", "tensor_tensor", dict(out=zsq[:], in0=zq[:], in1=zq[:], op=MUL), [zq_r], [zsq_r])
                      op("dve", "tensor_reduce", dict(out=hss[:], in_=zsq[:], axis=AX.X, op=ADD), [zsq_r], [hss_r])
                      op("act", "activation", dict(out=hrs[:], in_=hss[:], func=AF.Sqrt, scale=1.0 / 64, bias=epsb[:, 0:1]), [hss_r, epsb_r], [hrs_r])
                      op("dve", "reciprocal", dict(out=hrs[:], in_=hrs[:]), [hrs_r], [hrs_r])
                      op("dve", "tensor_tensor", dict(out=zn[:], in0=zq[:], in1=V(hrs[:], [[1, 26], [0, 64]]), op=MUL), [zq_r, hrs_r], [zn_r])
                      for gi, (h0, h1) in enumerate([(0, 8), (8, 16), (16, 24), (24, 26)]):
                          op("pool", "tensor_tensor", dict(out=zn[:, h0:h1, :], in0=zn[:, h0:h1, :], in1=V(G4[:, gi * 64:gi * 64 + 1], [[0, h1 - h0], [1, 64]]), op=MUL), [zn_r, G4_r], [zn_r])
                      op("pool", "tensor_tensor", dict(out=t1[:], in0=zn[:], in1=V(r_t[:, 0:1], [[0, 26], [1, 64]]), op=MUL), [zn_r, r_r], [t1_r])
                      op("pool", "tensor_tensor", dict(out=t2[:, :, 0:32], in0=zn[:, :, 32:64], in1=V(r_t[:, 64:65], [[0, 26], [1, 32]]), op=MUL), [zn_r, r_r], [t2_r])
                      op("pool", "tensor_tensor", dict(out=t2[:, :, 32:64], in0=zn[:, :, 0:32], in1=V(r_t[:, 96:97], [[0, 26], [1, 32]]), op=MUL), [zn_r, r_r], [t2_r])
                      op("dve", "tensor_tensor", dict(out=zr[:].rearrange("p (a b) -> p a b", b=64), in0=t1[:], in1=t2[:], op=ADD), [t1_r, t2_r], [zr_r])
                      sub = it % NSUB
                      q_t, q_r = qst[(it // NSUB) % 2]
                      s_t, s_r = sst[(it // NSUB) % 2]
                      for j in range(8):
                          tr(tqb[:, j, :], zr[:, j * 128:(j + 1) * 128], [zr_r], [tqb_r])
                      op("act", "activation", dict(out=q_t[:, :, sub * 128:(sub + 1) * 128], in_=tqb[:], func=AF.Copy), [tqb_r], [q_r])
                      for j in range(8):
                          tr(tsb[:, j, :], zr[:, 1024 + j * 64:1024 + (j + 1) * 64], [zr_r], [tsb_r])
                      op("dve", "tensor_copy", dict(out=s_t[:, 0:8, sub * 128:(sub + 1) * 128], in_=tsb[:]), [tsb_r], [s_r])
                      for j in range(2):
                          tr(tsb[:, j, :], zr[:, 1536 + j * 64:1536 + (j + 1) * 64], [zr_r], [tsb_r])
                      op("dve", "tensor_copy", dict(out=s_t[:, 8:10, sub * 128:(sub + 1) * 128], in_=tsb[:, 0:2, :]), [tsb_r], [s_r])
                      if samp:
                          dma("act", dict(out=Vs_in.ap()[:, :, lt * 128:(lt + 1) * 128].rearrange("h p e -> p h e"), in_=v_t[:].rearrange("p (h e) -> p h e", h=4)), v_r, "st", [v_r], [rs["SKV_in"]], merge=True)
                          dma("act", dict(out=SVs_in.ap()[:, :, lt * 128:(lt + 1) * 128].rearrange("h p e -> p h e"), in_=sv_t[:].rearrange("p a b c -> p a (b c)")), sv_r, "st", [sv_r], [rs["SKV_in"]], merge=True)
                      else:
                          dma("act", dict(out=V_d.ap()[lt * 128:(lt + 1) * 128, :], in_=v_t[:]), v_r, "st", [v_r], [rs["V_d"]], merge=True)
                          dma("act", dict(out=SV_d.ap()[lt * 128:(lt + 1) * 128, :], in_=sv_t[:].rearrange("p a b c -> p (a b c)")), sv_r, "st", [sv_r], [rs["SV_d"]], merge=True)
                      if sub == NSUB - 1:
                          tk0 = (tile - (NSUB - 1)) * 128
                          lk0 = (lt - (NSUB - 1)) * 128
                          nt = NSUB * 128
                          dma("sp", dict(out=QT_d.ap()[:, :, tk0:tk0 + nt].rearrange("h p t -> p h t"), in_=q_t[:, 0:4, :]), q_r, "st", [q_r], [rs["QT_d"]], merge=True)
                          dma("sp", dict(out=SQT_d.ap()[:, :, tk0:tk0 + nt].rearrange("h p t -> p h t"), in_=s_t[:, 0:8, :]), s_r, "st", [s_r], [rs["SQT_d"]], merge=True)
                          if samp:
                              dma("sp", dict(out=KTs_in.ap()[:, :, lk0:lk0 + nt].rearrange("h p t -> p h t"), in_=q_t[:, 4:8, :]), q_r, "st", [q_r], [rs["SKV_in"]], merge=True)
                              dma("sp", dict(out=SKs_in.ap()[:, :, lk0:lk0 + nt].rearrange("h p t -> p h t"), in_=s_t[:, 8:10, :]), s_r, "st", [s_r], [rs["SKV_in"]], merge=True)
                          else:
                              dma("sp", dict(out=KT_d.ap()[:, :, lk0:lk0 + nt].rearrange("h p t -> p h t"), in_=q_t[:, 4:8, :]), q_r, "st", [q_r], [rs["KT_d"]], merge=True)
                              dma("sp", dict(out=SKT_d.ap()[:, :, lk0:lk0 + nt].rearrange("h p t -> p h t"), in_=s_t[:, 8:10, :]), s_r, "st", [s_r], [rs["SKT_d"]], merge=True)
                      if it == TST - 1 and not cfg.get("nocc"):
                          for (ti, tg, nh) in ((KTs_in, KTs_g, 4), (Vs_in, Vs_g, 4), (SKs_in, SKs_g, 2), (SVs_in, SVs_g, 2)):
                              for hh in range(nh):
                                  S.custom("pool", "collective_compute", dict(kind="AllGather", op=ALU.bypass, replica_groups=[[0, 1, 2, 3], [4, 5, 6, 7]],
                                                                              ins=[ti.ap()[hh]], outs=[tg.ap()[hh]]), "CCKV", 1, [rs["SKV_in"]], [rs["SKV_g"]])
              if cfg.get("stop") == "A":
                  raise StopBuild()
              with S.scope():
                  Wout, Wout_r = S.sb("Wout", [128, 8, D], BF16)
                  stg = [S.sb("stgo%d" % i, [128, D], F32) for i in range(2)]
                  for c in range(8):
                      st_t, st_r = stg[c % 2]
                      dma("sp", dict(out=st_t[:], in_=w_out.ap()[l, c * 128:(c + 1) * 128, :]), st_r, "ld", [W], [st_r])
                      op("pool", "tensor_copy", dict(out=Wout[:, c, :], in_=st_t[:]), [st_r], [Wout_r])
                  mT, mT_r = S.sb("mT", [128, 8, SEG], BF16)
                  KTs = [S.sb("KTs%d" % i, [128, SEG], BF16) for i in range(2)]
                  Vs = [S.sb("Vs%d" % i, [128, KC, 128], BF16) for i in range(2)]
                  QTs = [S.sb("QTs%d" % i, [128, 512], BF16) for i in range(2)]
                  PTs = [S.sb("PTs%d" % i, [128, 512], BF16) for i in range(4)]
                  rinv = [S.sb("rinv%d" % i, [128, 512], F32) for i in range(2)]
                  av = [S.sb("av%d" % i, [128, 512], F32) for i in range(2)]
                  asq, asq_r = S.sb("asq", [128, 512], BF16)
                  rrs, rrs_r = S.sb("rrs", [128, 512], F32)
                  SKe, SKe_r = S.sb("SKe", [64, 2, SEG + 256], BF16)
                  SVe, SVe_r = S.sb("SVe", [128, KC + 2, 2, 128], BF16)
                  hK, hK_r = S.sb("hK", [64, 2, 4, 2, 128], BF16)
                  hV, hV_r = S.sb("hV", [128, 2, 4, 256], BF16)
                  SQs = [S.sb("SQs%d" % i, [64, 8, 512], BF16) for i in range(2)]
                  Rp, Rp_r = S.sb("Rp", [128, 512], F32)
                  xt2 = [S.sb("xd%d" % i, [128, D], F32) for i in range(2)]
                  ht = [S.sb("ht%d" % i, [128, D], F32) for i in range(2)]
                  junk2, junk2_r = S.sb("junk2", [128, D], F32)
                  ss2, ss2_r = S.sb("ss2", [128, 8], F32)
                  n2 = [S.sb("n2%d" % i, [128, D], BF16) for i in range(2)]
                  n2T, n2T_r = S.sb("n2T", [128, 8, 128], BF16)
                  ex, ex_r = S.sb("ex", [128, NE], F32)
                  ST, ST_r = S.ps("ST", [128, 2, 512], F32)
                  STr = [Res("ST0"), Res("ST1")]
                  OT, OT_r = S.ps("OT", [128, 2, 512], F32)
                  OTr = [Res("OT0"), Res("OT1")]
                  RT, RT_r = S.ps("RT", [128, 2, 512], F32)
                  RTr = [Res("RT0"), Res("RT1")]
                  XF, XF_r = S.ps("XF", [128, 512], F32)
                  XB, XB_r = S.ps("XB", [128, 8, 128], BF16)
                  pcnt = 0
                  segc = 0
                  qgc = 0
                  for u in range(NU):
                      samp = u == NU - 1
                      t0 = u * SEG
                      nseg = 4 if samp else 1
                      for h in range(4):
                          for qg in range(QG):
                              Q_t, Q_r = QTs[qgc % 2]
                              qgc += 1
                              dma("sp", dict(out=Q_t[:], in_=QT_d.ap()[h, :, t0 + qg * 512: t0 + (qg + 1) * 512]), Q_r, "ld", [rs["QT_d"]], [Q_r])
                              for sg in range(nseg):
                                  K_t, K_r = KTs[segc % 2]
                                  V_t, V_r = Vs[segc % 2]
                                  segc += 1
                                  if samp:
                                      ksrc = KTs_g.ap()[h, sg * 128:(sg + 1) * 128, :]
                                      vsrc = Vs_g.ap()[h, sg * 128:(sg + 1) * 128, :].rearrange("p (c e) -> p c e", e=128)
                                      kr, vr = rs["SKV_g"], rs["SKV_g"]
                                  else:
                                      ksrc = KT_d.ap()[h, :, t0:t0 + SEG]
                                      vsrc = V_d.ap()[t0:t0 + SEG, h * 128:(h + 1) * 128].rearrange("(c p) e -> p c e", p=128)
                                      kr, vr = rs["KT_d"], rs["V_d"]
                                  dma("sp", dict(out=K_t[:], in_=ksrc), K_r, "ld", [kr], [K_r])
                                  dma("sp", dict(out=V_t[:], in_=vsrc), V_r, "ld", [vr], [V_r])
                                  for kc in range(KC):
                                      first = sg == 0 and kc == 0
                                      last = sg == nseg - 1 and kc == KC - 1
                                      for c in range(2):
                                          mm(ST[:, c, :], K_t[c * 64:(c + 1) * 64, kc * 128:(kc + 1) * 128], Q_t[c * 64:(c + 1) * 64, :], True, True, [K_r, Q_r], [STr[c]])
                                          P_t, P_r = PTs[pcnt % 4]
                                          pcnt += 1
                                          op("act", "activation", dict(out=P_t[:], in_=ST[:, c, :], func=AF.Exp, scale=0.125), [STr[c]], [P_r])
                                          mm(OT[:, c, :], V_t[:, kc, :], P_t[:], first, last, [V_r, P_r], [OTr[c]])
                                          mm(RT[:, c, :], onesb[:], P_t[:], first, last, [onesb_r, P_r], [RTr[c]])
                              for c in range(2):
                                  ri_t, ri_r = rinv[c]
                                  op("dve", "reciprocal", dict(out=ri_t[:], in_=RT[:, c, :]), [RTr[c]], [ri_r])
                                  a_t, a_r = av[c]
                                  op("dve", "tensor_tensor", dict(out=a_t[:], in0=OT[:, c, :], in1=ri_t[:], op=MUL), [OTr[c], ri_r], [a_r])
                              op("dve", "scalar_tensor_tensor", dict(out=av[0][0][:], in0=av[1][0][:], scalar=lamneg[:, 0:1], in1=av[0][0][:], op0=MUL, op1=ADD), [av[0][1], av[1][1], lamneg_r], [av[0][1]])
                              op("act", "activation", dict(out=asq[:], in_=av[0][0][:], func=AF.Square), [av[0][1]], [asq_r])
                              mm(XF[:], onesb[:], asq[:], True, True, [onesb_r, asq_r], [XF_r])
                              op("act", "activation", dict(out=rrs[:], in_=XF[:], func=AF.Sqrt, scale=1.0 / 128, bias=epsb[:, 0:1]), [XF_r, epsb_r], [rrs_r])
                              op("dve", "reciprocal", dict(out=rrs[:], in_=rrs[:]), [rrs_r], [rrs_r])
                              op("dve", "scalar_tensor_tensor", dict(out=mT[:, h, qg * 512:(qg + 1) * 512], in0=av[0][0][:], scalar=sublnS[:, 0:1], in1=rrs[:], op0=MUL, op1=MUL), [av[0][1], sublnS_r, rrs_r], [mT_r])
                      if samp:
                          dma("sp", dict(out=SKe[:, :, 128:128 + SEG], in_=SKs_in.ap().rearrange("h p t -> p h t")), SKe_r, "ld", [rs["SKV_in"]], [SKe_r])
                          for hh in range(2):
                              dma("sp", dict(out=SVe[:, 1:KC + 1, hh, :], in_=SVs_in.ap()[hh].rearrange("p (c e) -> p c e", e=128)), SVe_r, "ld", [rs["SKV_in"]], [SVe_r])
                          for r in range(4):
                              gkr = SKs_g.ap()[:, r * 64:(r + 1) * 64, :].rearrange("h p t -> p h t")
                              dma("sp", dict(out=hK[:, 0, r], in_=gkr[:, :, SEG - 128:SEG]), hK_r, "ld", [rs["SKV_g"]], [hK_r])
                              dma("sp", dict(out=hK[:, 1, r], in_=gkr[:, :, 0:128]), hK_r, "ld", [rs["SKV_g"]], [hK_r])
                          for hh in range(2):
                              gv = SVs_g.ap()[hh].rearrange("(r p) c -> p r c", p=128)
                              dma("sp", dict(out=hV[:, 0, :, hh * 128:(hh + 1) * 128], in_=gv[:, :, (KC - 1) * 128:KC * 128]), hV_r, "ld", [rs["SKV_g"]], [hV_r])
                              dma("sp", dict(out=hV[:, 1, :, hh * 128:(hh + 1) * 128], in_=gv[:, :, 0:128]), hV_r, "ld", [rs["SKV_g"]], [hV_r])
                          for side in range(2):
                              kdst = SKe[:, :, 0:128] if side == 0 else SKe[:, :, 128 + SEG:256 + SEG]
                              vdst = (SVe[:, 0, :, :] if side == 0 else SVe[:, KC + 1, :, :]).rearrange("p h d -> p (h d)")
                              for r in range(4):
                                  wcol = cc_t[:, 2 + side * 4 + r: 3 + side * 4 + r]
                                  if r == 0:
                                      op("dve", "tensor_scalar", dict(out=kdst, in0=hK[:, side, r], scalar1=wcol[0:64, :], scalar2=None, op0=MUL), [hK_r, cc_r], [SKe_r])
                                      op("dve", "tensor_scalar", dict(out=vdst, in0=hV[:, side, r], scalar1=wcol, scalar2=None, op0=MUL), [hV_r, cc_r], [SVe_r])
                                  else:
                                      op("dve", "scalar_tensor_tensor", dict(out=kdst, in0=hK[:, side, r], scalar=wcol[0:64, :], in1=kdst, op0=MUL, op1=ADD), [hK_r, cc_r, SKe_r], [SKe_r])
                                      op("dve", "scalar_tensor_tensor", dict(out=vdst, in0=hV[:, side, r], scalar=wcol, in1=vdst, op0=MUL, op1=ADD), [hV_r, cc_r, SVe_r], [SVe_r])
                      else:
                          dma("sp", dict(out=SKe[:, :, 128:128 + SEG], in_=SKT_d.ap()[:, :, t0:t0 + SEG].rearrange("h p t -> p h t")), SKe_r, "ld", [rs["SKT_d"]], [SKe_r])
                          dma("sp", dict(out=SVe[:, 1:KC + 1, :, :].rearrange("p c h d -> p c (h d)"), in_=SV_d.ap()[t0:t0 + SEG, :].rearrange("(c p) e -> p c e", p=128)), SVe_r, "ld", [rs["SV_d"]], [SVe_r])
                      for n in range(KC):
                          if n % 4 == 0:
                              SQ_t, SQ_r = SQs[(n // 4) % 2]
                              dma("sp", dict(out=SQ_t[:], in_=SQT_d.ap()[:, :, t0 + n * 128: t0 + n * 128 + 512].rearrange("h p t -> p h t")), SQ_r, "ld", [rs["SQT_d"]], [SQ_r])
                          for h in range(2):
                              js = [j for j in range(3) if samp or (0 < n + j < KC + 1)]
                              for ji, j in enumerate(js):
                                  ci = n + j
                                  c = pcnt % 2
                                  mm(ST[:, c, :], SKe[:, h, ci * 128:(ci + 1) * 128], SQ_t[:, h * 4:(h + 1) * 4, (n % 4) * 128:(n % 4 + 1) * 128], True, True, [SKe_r, SQ_r], [STr[c]])
                                  P_t, P_r = PTs[pcnt % 4]
                                  pcnt += 1
                                  op("act", "activation", dict(out=P_t[:], in_=ST[:, c, :], func=AF.Exp, scale=0.125), [STr[c]], [P_r])
                                  if j != 1:
                                      if j == 0:
                                          mk_t, mk_r = (triLe, triLe_r) if (samp and n == 0) else (triL, triL_r)
                                      else:
                                          mk_t, mk_r = (triRe, triRe_r) if (samp and n == KC - 1) else (triR, triR_r)
                                      op("dve", "tensor_tensor", dict(out=P_t[:].rearrange("p (g q) -> p g q", g=4), in0=P_t[:].rearrange("p (g q) -> p g q", g=4), in1=V(mk_t[:, 0:1], [[0, 4], [1, 128]]), op=MUL), [P_r, mk_r], [P_r])
                                  mm(OT[:, 0, :], SVe[:, ci, h, :], P_t[:], ji == 0, ji == len(js) - 1, [SVe_r, P_r], [OTr[0]])
                                  mm(RT[:, 0, :], onesb[:], P_t[:], ji == 0, ji == len(js) - 1, [onesb_r, P_r], [RTr[0]])
                              op("dve", "tensor_tensor", dict(out=Rp[:].rearrange("p (g q) -> p g q", g=4), in0=RT[:, 0, :].rearrange("p (g q) -> p g q", g=4), in1=V(esink[:, h * 4:h * 4 + 1], [[1, 4], [0, 128]]), op=ADD), [RTr[0], esink_r], [Rp_r])
                              op("dve", "reciprocal", dict(out=Rp[:], in_=Rp[:]), [Rp_r], [Rp_r])
                              for pp in range(2):
                                  o_in = V(OT[pp * 64:(pp + 1) * 64, 0, pp * 128:pp * 128 + 1], [[256, 2], [1, 128]])
                                  r_in = V(Rp[pp * 64:(pp + 1) * 64, pp * 128:pp * 128 + 1], [[256, 2], [1, 128]])
                                  m_out = mT[pp * 64:(pp + 1) * 64, 4 + h * 2:4 + h * 2 + 2, n * 128:(n + 1) * 128]
                                  op("dve", "tensor_tensor", dict(out=m_out, in0=o_in, in1=r_in, op=MUL), [OTr[0], Rp_r], [mT_r])
                      for n in range(KC):
                          tile = u * KC + n
                          x_t, x_r = xt2[n % 2]
                          h_t, h_r = ht[n % 2]
                          n_t, n_r = n2[n % 2]
                          src, src_r = xsrc_ap(l, tile)
                          dma("sp", dict(out=x_t[:], in_=src), x_r, "ld", [src_r], [x_r])
                          for half in range(2):
                              for c in range(8):
                                  mm(OT[:, half, :], mT[:, c, n * 128:(n + 1) * 128], Wout[:, c, half * 512:(half + 1) * 512], c == 0, c == 7, [mT_r, Wout_r], [OTr[half]])
                          op("dve", "tensor_tensor", dict(out=h_t[:].rearrange("p (a b) -> p a b", a=2), in0=OT[:], in1=x_t[:].rearrange("p (a b) -> p a b", a=2), op=ADD), [OTr[0], OTr[1], x_r], [h_r])
                          dma("act", dict(out=H_d.ap()[tile * 128:(tile + 1) * 128, :], in_=h_t[:]), h_r, "st", [h_r], [rs["H_d"]], merge=True)
                          op("act", "activation", dict(out=junk2[:], in_=h_t[:], func=AF.Square, accum_out=ss2[:, 0:1]), [h_r], [junk2_r, ss2_r])
                          op("act", "activation", dict(out=ss2[:, 1:2], in_=ss2[:, 0:1], func=AF.Sqrt, scale=1.0 / D, bias=epsb[:, 0:1]), [ss2_r, epsb_r], [ss2_r])
                          op("dve", "reciprocal", dict(out=ss2[:, 2:3], in_=ss2[:, 1:2]), [ss2_r], [ss2_r])
                          op("dve", "scalar_tensor_tensor", dict(out=n_t[:], in0=h_t[:], scalar=ss2[:, 2:3], in1=g_ffn[:], op0=MUL, op1=MUL), [h_r, ss2_r, g_ffn_r], [n_r])
                          dma("act", dict(out=XN_d.ap()[tile * 128:(tile + 1) * 128, :], in_=n_t[:]), n_r, "st", [n_r], [rs["XN_d"]], merge=True)
                          for c in range(8):
                              tr(XB[:, c, :], n_t[:, c * 128:(c + 1) * 128], [n_r], [XB_r])
                          op("act", "activation", dict(out=n2T[:], in_=XB[:], func=AF.Copy), [XB_r], [n2T_r])
                          for c in range(8):
                              mm(XF[:, 0:NE], n2T[:, c, :], wrb[:, c, :], c == 0, c == 7, [n2T_r, wrb_r], [XF_r])
                          op("dve", "tensor_reduce", dict(out=ss2[:, 3:4], in_=XF[:, 0:NE], axis=AX.X, op=ALU.max), [XF_r], [ss2_r])
                          op("dve", "tensor_scalar", dict(out=ss2[:, 4:5], in0=ss2[:, 3:4], scalar1=-1.0, scalar2=None, op0=MUL), [ss2_r], [ss2_r])
                          op("act", "activation", dict(out=ex[:], in_=XF[:, 0:NE], func=AF.Exp, bias=ss2[:, 4:5], accum_out=ss2[:, 5:6]), [XF_r, ss2_r], [ex_r, ss2_r])
                          op("dve", "reciprocal", dict(out=ss2[:, 6:7], in_=ss2[:, 5:6]), [ss2_r], [ss2_r])
                          op("dve", "tensor_scalar", dict(out=AFFS[:, tile, :], in0=ex[:], scalar1=ss2[:, 6:7], scalar2=None, op0=MUL), [ex_r, ss2_r], [AFFS_r])
              if cfg.get("stop") == "BD":
                  raise StopBuild()
              with S.scope():
                  dma("sp", dict(out=AFF_d.ap(), in_=AFFS[:].rearrange("p t e -> p (t e)")), AFFS_r, "st", [AFFS_r], [rs["AFF_d"]])
                  S.custom("pool", "collective_compute", dict(kind="AllGather", op=ALU.bypass, replica_groups=[[0, 1, 2, 3, 4, 5, 6, 7]],
                                                              ins=[AFF_d.ap()], outs=[AFFG_d.ap()]), "CCAFF", 1, [rs["AFF_d"]], [rs["AFFG_d"]])
                  NTmax = max(TPT, TST)
                  Ag, Ag_r = S.sb("Ag", [128, 8, NTmax * NE], F32)
                  cmpb, cmpb_r = S.sb("cmpb", [128, 8 * NTmax * NE], BF16)
                  cntp, cntp_r = S.sb("cntp", [128, 2, NE], BF16)
                  cntf, cntf_r = S.sb("cntf", [128, 2, NE], F32)
                  lo, lo_r = S.sb("lo", [128, NE], F32)
                  hi, hi_r = S.sb("hi", [128, NE], F32)
                  mid, mid_r = S.sb("mid", [128, NE], F32)
                  ge, ge_r = S.sb("ge", [128, NE], F32)
                  dd, dd_r = S.sb("dd", [128, NE], F32)
                  thr = [S.sb("thr%d" % i, [128, NE], F32) for i in range(2)]
                  maskb, maskb_r = S.sb("maskb", [128, TT, NE], BF16)
                  pcn, pcn_r = S.ps("pcn", [128, NE], F32)
                  cps, cps_r = S.ps("cps", [128, TT * NE], F32)
                  for grp in range(2):
                      tl0, nt = (0, TPT) if grp == 0 else (TPT, TST)
                      n_el = 8 * nt
                      dma("sp", dict(out=Ag[:, :, 0:nt * NE], in_=AFFG_d.ap().rearrange("(r p) c -> p r c", p=128)[:, :, tl0 * NE:(tl0 + nt) * NE]), Ag_r, "ld", [rs["AFFG_d"]], [Ag_r])
                      op("dve", "memset", dict(ap=lo[:], constant=0.0), [], [lo_r])
                      op("dve", "memset", dict(ap=hi[:], constant=1.0), [], [hi_r])
                      a_in = V(Ag[:, 0, 0:1], [[NTmax * NE, 8], [NE, nt], [1, NE]])
                      c_out = V(cmpb[:, 0:1], [[nt * NE, 8], [NE, nt], [1, NE]])
                      c_red = V(cmpb[:, 0:1], [[4 * nt * NE, 2], [1, NE], [NE, 4 * nt]])
                      for itn in range(NITER):
                          op("dve", "tensor_tensor", dict(out=mid[:], in0=lo[:], in1=hi[:], op=ADD), [lo_r, hi_r], [mid_r])
                          op("dve", "tensor_scalar", dict(out=mid[:], in0=mid[:], scalar1=0.5, scalar2=None, op0=MUL), [mid_r], [mid_r])
                          op("dve", "tensor_tensor", dict(out=c_out, in0=a_in, in1=V(mid[:, 0:1], [[0, 8], [0, nt], [1, NE]]), op=ALU.is_ge), [Ag_r, mid_r], [cmpb_r])
                          op("dve", "tensor_reduce", dict(out=cntf[:], in_=c_red, axis=AX.X, op=ADD), [cmpb_r], [cntf_r])
                          op("dve", "tensor_copy", dict(out=cntp[:], in_=cntf[:]), [cntf_r], [cntp_r])
                          mm(pcn[:], onesb[:], cntp[:, 0, :], True, False, [onesb_r, cntp_r], [pcn_r])
                          mm(pcn[:], onesb[:], cntp[:, 1, :], False, True, [onesb_r, cntp_r], [pcn_r])
                          op("dve", "tensor_scalar", dict(out=ge[:], in0=pcn[:], scalar1=float(CAPS[grp]) - 0.5, scalar2=None, op0=ALU.is_ge), [pcn_r], [ge_r])
                          op("dve", "tensor_tensor", dict(out=dd[:], in0=mid[:], in1=lo[:], op=SUB), [mid_r, lo_r], [dd_r])
                          op("dve", "tensor_tensor", dict(out=dd[:], in0=dd[:], in1=ge[:], op=MUL), [dd_r, ge_r], [dd_r])
                          op("dve", "tensor_tensor", dict(out=lo[:], in0=lo[:], in1=dd[:], op=ADD), [lo_r, dd_r], [lo_r])
                          op("dve", "tensor_tensor", dict(out=dd[:], in0=hi[:], in1=mid[:], op=SUB), [mid_r, hi_r], [dd_r])
                          op("dve", "tensor_tensor", dict(out=dd[:], in0=dd[:], in1=ge[:], op=MUL), [dd_r, ge_r], [dd_r])
                          op("dve", "tensor_tensor", dict(out=hi[:], in0=mid[:], in1=dd[:], op=ADD), [mid_r, dd_r], [hi_r])
                      op("dve", "tensor_copy", dict(out=thr[grp][0][:], in_=lo[:]), [lo_r], [thr[grp][1]])
                      op("dve", "tensor_tensor", dict(out=maskb[:, tl0:tl0 + nt, :], in0=AFFS[:, tl0:tl0 + nt, :], in1=V(thr[grp][0][:, 0:1], [[0, nt], [1, NE]]), op=ALU.is_ge), [AFFS_r, thr[grp][1]], [maskb_r])
                  for b in range(NB):
                      for j, t in enumerate(BT[b]):
                          mm(cps[:, t * NE:(t + 1) * NE], Utri[:], maskb[:, t, :], True, j == 0, [Utri_r, maskb_r], [cps_r])
                          for jj in range(j):
                              mm(cps[:, t * NE:(t + 1) * NE], onesb[:], maskb[:, BT[b][jj], :], False, jj == j - 1, [onesb_r, maskb_r], [cps_r])
                  op("dve", "tensor_tensor", dict(out=posm[:].rearrange("p t e -> p (t e)"), in0=cps[:], in1=maskb[:].rearrange("p t e -> p (t e)"), op=MUL), [cps_r, maskb_r], [posm_r])
                  op("dve", "tensor_scalar", dict(out=posm[:], in0=posm[:], scalar1=-1.0, scalar2=None, op0=ADD), [posm_r], [posm_r])

              if cfg.get("stop") == "E":
                  raise StopBuild()
              with S.scope():
                  Wg = [S.sb("Wg%d" % i, [128, 8, D], BF16) for i in range(2)]
                  Wu = [S.sb("Wu%d" % i, [128, 8, D], BF16) for i in range(2)]
                  Wd = [S.sb("Wd%d" % i, [128, 8, D], BF16) for i in range(2)]
                  stg = [S.sb("stgf%d" % i, [128, D], F32) for i in range(4)]
                  XNb = [S.sb("XNb%d" % i, [128, 4, D], BF16) for i in range(2)]
                  sel = [S.sb("sel%d" % i, [128, CAPT, 4, 128], BF16) for i in range(2)]
                  XET = [S.sb("XET%d" % i, [128, 8, NS], BF16) for i in range(2)]
                  hT, hT_r = S.sb("hT", [128, 8, NS], BF16)
                  sg_ = [S.sb("sg%d" % i, [128, NS], F32) for i in range(2)]
                  ye = [S.sb("ye%d" % i, [128, D], BF16) for i in range(2)]
                  gps, gps_r = S.ps("gps", [128, 8, 128], F32)
                  gu = [S.ps("gu%d" % i, [128, 2, 512], F32) for i in range(2)]
                  yps, yps_r = S.ps("yps", [128, 2, 512], F32)
                  stc = 0
                  bcn = 0
                  gcn = 0
                  fcn = 0
                  ycn = 0

                  def load_w(e):
                      nonlocal stc
                      for (wsrc, wdst) in ((w_gate, Wg), (w_up, Wu), (w_down, Wd)):
                          w_t, w_r = wdst[e % 2]
                          for c in range(8):
                              st_t, st_r = stg[stc % 4]
                              stc += 1
                              dma("sp", dict(out=st_t[:], in_=wsrc.ap()[l, e, c * 128:(c + 1) * 128, :]), st_r, "ld", [W], [st_r])
                              op("pool", "tensor_copy", dict(out=w_t[:, c, :], in_=st_t[:]), [st_r], [w_r])
                  load_w(0)
                  for e in range(NE):
                      if e + 1 < NE:
                          load_w(e + 1)
                      Wg_t, Wg_r = Wg[e % 2]
                      Wu_t, Wu_r = Wu[e % 2]
                      Wd_t, Wd_r = Wd[e % 2]
                      for gi in range(NB // G):
                          X_t, X_r = XET[gcn % 2]
                          gcn += 1
                          for bi in range(G):
                              b = gi * G + bi
                              B_t, B_r = XNb[bcn % 2]
                              s_t, s_r = sel[bcn % 2]
                              bcn += 1
                              for t in range(4):
                                  tl = BT[b][t]
                                  dma("act", dict(out=B_t[:, t, :], in_=XN_d.ap()[tl * 128:(tl + 1) * 128, :]), B_r, "ld", [rs["XN_d"]], [B_r])
                              for s_ in range(CAPT):
                                  for t in range(4):
                                      op("dve", "tensor_scalar", dict(out=s_t[:, s_, t, :], in0=iota2[:, s_, :], scalar1=posm[:, BT[b][t], e:e + 1], scalar2=None, op0=ALU.is_equal), [iota2_r, posm_r], [s_r])
                              for s_ in range(CAPT):
                                  for c in range(8):
                                      for t in range(4):
                                          mm(gps[:, c, :], B_t[:, t, c * 128:(c + 1) * 128], s_t[:, s_, t, :], t == 0, t == 3, [B_r, s_r], [gps_r])
                                  so = (bi * CAPT + s_) * 128
                                  if s_ % 2 == 0:
                                      op("act", "activation", dict(out=X_t[:, :, so:so + 128], in_=gps[:], func=AF.Copy), [gps_r], [X_r])
                                  else:
                                      op("dve", "tensor_copy", dict(out=X_t[:, :, so:so + 128], in_=gps[:]), [gps_r], [X_r])
                          for fc in range(8):
                              gu_t, gu_r = gu[fcn % 2]
                              s_t2, s_r2 = sg_[fcn % 2]
                              fcn += 1
                              for c in range(8):
                                  mm(gu_t[:, 0, 0:NS], Wg_t[:, c, fc * 128:(fc + 1) * 128], X_t[:, c, :], c == 0, c == 7, [Wg_r, X_r], [gu_r])
                              for c in range(8):
                                  mm(gu_t[:, 1, 0:NS], Wu_t[:, c, fc * 128:(fc + 1) * 128], X_t[:, c, :], c == 0, c == 7, [Wu_r, X_r], [gu_r])
                              op("act", "activation", dict(out=s_t2[:], in_=gu_t[:, 0, 0:NS], func=AF.Silu), [gu_r], [s_r2])
                              op("dve", "tensor_tensor", dict(out=hT[:, fc, :], in0=gu_t[:, 1, 0:NS], in1=s_t2[:], op=MUL), [gu_r, s_r2], [hT_r])
                          for sti in range(G * CAPT):
                              b = gi * G + sti // CAPT
                              s_ = sti % CAPT
                              for half in range(2):
                                  for fc in range(8):
                                      mm(yps[:, half, :], hT[:, fc, sti * 128:(sti + 1) * 128], Wd_t[:, fc, half * 512:(half + 1) * 512], fc == 0, fc == 7, [hT_r, Wd_r], [yps_r])
                              y_t, y_r = ye[ycn % 2]
                              ycn += 1
                              if ycn % 2:
                                  op("act", "activation", dict(out=y_t[:].rearrange("p (a b) -> p a b", a=2), in_=yps[:], func=AF.Copy), [yps_r], [y_r])
                              else:
                                  op("dve", "tensor_copy", dict(out=y_t[:].rearrange("p (a b) -> p a b", a=2), in_=yps[:]), [yps_r], [y_r])
                              dma("sp", dict(out=YE_d.ap()[b, e, s_], in_=y_t[:]), y_r, "st", [y_r], [rs["YE_d"]], merge=True)

              if cfg.get("stop") == "F1":
                  raise StopBuild()
              with S.scope():
                  NES = NE * CAPT
                  YEb = [S.sb("YEb%d" % i, [128, NES, D], BF16) for i in range(2)]
                  selg = [S.sb("selg%d" % i, [128, NE, CAPT * 128], BF16) for i in range(1)]
                  selT = [S.sb("selT%d" % i, [128, NES, 128], BF16) for i in range(2)]
                  hin = [S.sb("hin%d" % i, [128, D], F32) for i in range(2)]
                  xo = [S.sb("xo%d" % i, [128, D], F32) for i in range(2)]
                  sps, sps_r = S.ps("sps", [128, NES, 128], BF16)
                  yo = [S.ps("yo%d" % i, [128, 2, 512], F32) for i in range(2)]
                  for b in range(NB):
                      Y_t, Y_r = YEb[b % 2]
                      for e4 in range(4):
                          dma("act", dict(out=Y_t[:, e4 * 4 * CAPT:(e4 + 1) * 4 * CAPT, :], in_=YE_d.ap()[b, e4 * 4:(e4 + 1) * 4].rearrange("e s p d -> p (e s) d")), Y_r, "ld", [rs["YE_d"]], [Y_r])
                      for t in range(4):
                          tile = BT[b][t]
                          k = t % 2
                          sg_t, sg_r = selg[0]
                          sT_t, sT_r = selT[k]
                          h_t, h_r = hin[k]
                          o_t, o_r = xo[k]
                          yo_t, yo_r = yo[k]
                          dma("sp", dict(out=h_t[:], in_=H_d.ap()[tile * 128:(tile + 1) * 128, :]), h_r, "ld", [rs["H_d"]], [h_r])
                          op("dve", "tensor_tensor", dict(out=sg_t[:], in0=V(iota2[:, 0, 0:1], [[0, NE], [1, CAPT * 128]]), in1=V(posm[:, tile, 0:1], [[1, NE], [0, CAPT * 128]]), op=ALU.is_equal), [iota2_r, posm_r], [sg_r])
                          op("dve", "tensor_tensor", dict(out=sg_t[:], in0=sg_t[:], in1=V(AFFS[:, tile, 0:1], [[1, NE], [0, CAPT * 128]]), op=MUL), [sg_r, AFFS_r], [sg_r])
                          for es in range(NES):
                              tr(sps[:, es, :], sg_t[:, es // CAPT, (es % CAPT) * 128:(es % CAPT + 1) * 128], [sg_r], [sps_r])
                          op("act", "activation", dict(out=sT_t[:], in_=sps[:], func=AF.Copy), [sps_r], [sT_r])
                          for half in range(2):
                              for es in range(NES):
                                  mm(yo_t[:, half, :], sT_t[:, es, :], Y_t[:, es, half * 512:(half + 1) * 512], es == 0, es == NES - 1, [sT_r, Y_r], [yo_r])
                          op("dve", "tensor_tensor", dict(out=o_t[:].rearrange("p (a b) -> p a b", a=2), in0=yo_t[:], in1=h_t[:].rearrange("p (a b) -> p a b", a=2), op=ADD), [yo_r, h_r], [o_r])
                          dst, dst_r = xdst_ap(l, tile)
                          dma("sp", dict(out=dst, in_=o_t[:]), o_r, "st", [o_r], [dst_r], merge=True)
          except StopBuild:
            break
        S.barrier()
        S.emit()
    return nc


def _host_consts(SEG):
    p = np.arange(128)
    cst = np.zeros((128, 640), np.float32)
    cst[:, 0:128] = (p[:, None] == p[None, :])
    cst[:, 128:256] = (p[:, None] <= p[None, :])
    cst[:, 256:384] = (p[:, None] >= p[None, :])
    cst[:, 384:512] = (p[:, None] <= p[None, :])
    cst[:, 512:640] = p[None, :].astype(np.float32)
    return cst


def _rope_table(positions):
    d = 64
    inv = (1.0 / (np.float32(10000.0) ** (np.arange(0, d, 2, dtype=np.float32) / np.float32(d)))).astype(np.float32)
    ang = positions.astype(np.float32)[:, None] * inv[None, :]
    c = np.cos(ang).astype(np.float32)
    s = np.sin(ang).astype(np.float32)
    return np.concatenate([c, c, -s, s], axis=1).astype(np.float32)


_NC_CACHE = {}


def run_cfg(inputs, L, NPS, SEG):
    key = (L, NPS, SEG)
    if key not in _NC_CACHE:
        _NC_CACHE[key] = build(dict(L=L, NPS=NPS, SEG=SEG))
    nc = _NC_CACHE[key]
    f = lambda k: np.ascontiguousarray(np.asarray(inputs[k], dtype=np.float32))
    x_prompt, x_sample = f("x_prompt"), f("x_sample")
    g4 = np.concatenate([f("diff_q_norm"), f("diff_k_norm"), f("swa_q_norm"), f("swa_k_norm")], axis=1)
    lam4 = np.concatenate([f("lambda_q1"), f("lambda_k1"), f("lambda_q2"), f("lambda_k2")], axis=1)
    cst = _host_consts(SEG)
    shared = dict(cst=cst, attn_norm=f("attn_norm"), w_in=f("w_in"), g4=np.ascontiguousarray(g4), lam4=np.ascontiguousarray(lam4),
                  subln=f("diff_subln"), sink=f("swa_sink"), w_out=f("w_out"), ffn_norm=f("ffn_norm"),
                  w_router=f("w_router"), w_gate=f("w_gate"), w_up=f("w_up"), w_down=f("w_down"))
    in_maps = []
    for i in range(8):
        seq, pos = i // 4, i % 4
        xp = x_prompt[i * NPS:(i + 1) * NPS].reshape(NPS * SEG, D)
        xs = x_sample[seq, pos * SEG:(pos + 1) * SEG]
        positions = np.concatenate([np.tile(np.arange(SEG), NPS), pos * SEG + np.arange(SEG)])
        cc = np.zeros((128, 16), np.float32)
        cc[:, 0] = 1.0 if pos > 0 else 0.0
        cc[:, 1] = 1.0 if pos < 3 else 0.0
        if pos > 0:
            cc[:, 2 + pos - 1] = 1.0
        if pos < 3:
            cc[:, 6 + pos + 1] = 1.0
        m = dict(shared)
        m.update(xp=np.ascontiguousarray(xp), xs=np.ascontiguousarray(xs), ropet=_rope_table(positions), cct=cc)
        in_maps.append(m)
    res = run_bass_kernel_spmd(nc, in_maps, core_ids=list(range(8)))
    y_prompt = np.zeros_like(x_prompt)
    y_sample = np.zeros_like(x_sample)
    for i in range(8):
        seq, pos = i // 4, i % 4
        r = res.results[i]
        y_prompt[i * NPS:(i + 1) * NPS] = np.asarray(r["yp"]).reshape(NPS, SEG, D)
        y_sample[seq, pos * SEG:(pos + 1) * SEG] = np.asarray(r["ys"])
    return y_prompt, y_sample


def kernel(**inputs):
    return run_cfg(inputs, 4, 4, 2048)
```

```python
import math
import numpy as np
from concourse.bass_utils import run_bass_kernel_spmd
import contextlib
import concourse.bass as bass
import concourse.mybir as mybir

F32 = mybir.dt.float32
BF16 = mybir.dt.bfloat16
I32 = mybir.dt.int32
AF = mybir.ActivationFunctionType
ALU = mybir.AluOpType
AX = mybir.AxisListType


class Res:
    __slots__ = ("name", "w", "r", "ld", "st", "depth")

    def __init__(self, name):
        self.name = name
        self.w = {}
        self.r = {}
        self.ld = None
        self.st = None
        self.depth = 0


class Sched:
    ENG = ("pe", "act", "dve", "pool", "sp")

    def __init__(self, nc, stack):
        self.nc = nc
        self.stack = stack
        self.root = stack
        self.scopes = []
        self.free_sems = []
        self.q = {e: [] for e in self.ENG}
        self.semh = {}
        self.semc = {}
        self.known = {e: {} for e in self.ENG}
        self.esem = {}
        for e in ("pe", "act", "dve", "pool"):
            self.esem[e] = self.newsem("E_" + e)
        self.nsem = 0
        self.uid = 0
        self.ckeys = {}
        self.ninstr = 0

    def newsem(self, key, persistent=False):
        if self.free_sems and not key.startswith("E_"):
            k = self.free_sems.pop()
        else:
            k = "s%d_%s" % (len(self.semh), key[:20])
            self.semh[k] = self.root.enter_context(self.nc.semaphore(k))
            self.semc[k] = 0
        if self.scopes and not persistent:
            self.scopes[-1][1].append(k)
        return k

    @contextlib.contextmanager
    def scope(self):
        self.barrier()
        st = contextlib.ExitStack()
        self.scopes.append((st, []))
        old = self.stack
        self.stack = st
        try:
            yield
        finally:
            self.barrier()
            self.stack = old
            _, sems = self.scopes.pop()
            self.free_sems.extend(sems)
            st.close()

    def sb(self, name, shape, dt):
        self.uid += 1
        name = "%s_%d" % (name, self.uid)
        t = self.stack.enter_context(self.nc.sbuf_tensor(name, list(shape), dt))
        r = Res(name)
        r.depth = len(self.scopes)
        return t, r

    def ps(self, name, shape, dt=F32):
        self.uid += 1
        name = "%s_%d" % (name, self.uid)
        t = self.stack.enter_context(self.nc.psum_tensor(name, list(shape), dt))
        r = Res(name)
        r.depth = len(self.scopes)
        return t, r

    def _collect(self, eng, reads, writes):
        waits = {}

        def need(d):
            for k, v in d.items():
                if waits.get(k, 0) < v:
                    waits[k] = v
        own = self.esem.get(eng)
        for r in reads:
            need(r.w)
        for w in writes:
            need(w.w)
            for k, v in w.r.items():
                if k == own:
                    continue
                if waits.get(k, 0) < v:
                    waits[k] = v
        kn = self.known[eng]
        wl = []
        for k, v in waits.items():
            if eng == "pe" and k == own:
                continue
            if kn.get(k, 0) >= v:
                continue
            kn[k] = v
            wl.append((k, v))
        return wl

    def op(self, eng, name, kw, reads=(), writes=()):
        fn = (name, kw)
        wl = self._collect(eng, reads, writes)
        k = self.esem[eng]
        self.semc[k] += 1
        c = self.semc[k]
        for r in reads:
            if r.r.get(k, 0) < c:
                r.r[k] = c
        for w in writes:
            w.w = {k: c}
            w.r = {}
        self.q[eng].append((wl, fn, k, 1))
        self.ninstr += 1

    def dma(self, qeng, kw, sb_res, direction, reads=(), writes=(), merge=False):
        fn = ("dma_start", kw)
        if direction == "ld":
            if sb_res.ld is None:
                sb_res.ld = self.newsem("L_" + sb_res.name, persistent=(sb_res.depth == 0))
            key = sb_res.ld
        else:
            if sb_res.st is None:
                sb_res.st = self.newsem("S_" + sb_res.name, persistent=(sb_res.depth == 0))
            key = sb_res.st
        saved = []
        for w in writes:
            if merge:
                saved.append(dict(w.w))
                w.w = {}
            elif key in w.w:
                w.w.pop(key)
        wl = self._collect(qeng, reads, writes)
        self.semc[key] += 16
        c = self.semc[key]
        for r in reads:
            if r.r.get(key, 0) < c:
                r.r[key] = c
        for i, w in enumerate(writes):
            if merge:
                old = saved[i]
                old[key] = c
                w.w = old
            else:
                w.w = {key: c}
                w.r = {}
        self.q[qeng].append((wl, fn, key, 16))
        self.ninstr += 1

    def custom(self, eng, name, kw, key, inc, reads=(), writes=()):
        fn = (name, kw)
        if key not in self.ckeys:
            self.ckeys[key] = self.newsem(key, persistent=True)
        key = self.ckeys[key]
        wl = self._collect(eng, reads, writes)
        self.semc[key] += inc
        c = self.semc[key]
        for r in reads:
            if r.r.get(key, 0) < c:
                r.r[key] = c
        for w in writes:
            w.w = {key: c}
            w.r = {}
        self.q[eng].append((wl, fn, key, inc))
        self.ninstr += 1

    def barrier(self):
        allv = {k: v for k, v in self.semc.items() if v > 0}
        for e in self.ENG:
            kn = self.known[e]
            wl = []
            for k, v in allv.items():
                if kn.get(k, 0) >= v:
                    continue
                kn[k] = v
                wl.append((k, v))
            if wl:
                self.q[e].append((wl, None, None, 0))

    def emit(self):
        nc = self.nc
        engs = {"pe": "tensor", "act": "scalar", "dve": "vector", "pool": "gpsimd", "sp": "sync"}
        with nc.Block() as block:
            for e in self.ENG:
                items = self.q[e]
                if not items:
                    continue

                def body(eng, items=items):
                    for wl, fn, key, inc in items:
                        for k, v in wl:
                            eng.wait_ge(self.semh[k], v)
                        if fn is not None:
                            ins = getattr(eng, fn[0])(**fn[1])
                            ins.then_inc(self.semh[key], inc)
                getattr(block, engs[e])(body)


def bc(ap_tensor, offset, dims):
    return bass.AP(ap_tensor, offset, [list(d) for d in dims])
D = 1024
NE = 16
INW = 2304
EPS = 1e-6
NITER = 24


def AP(t, off, dims):
    return bass.AP(t, off, [list(d) for d in dims])


def V(a, dims):
    return bass.AP(a.tensor, a.offset, [list(a.ap[0])] + [list(d) for d in dims])


class StopBuild(Exception):
    pass


def build(cfg):
    L, NPS, SEG = cfg["L"], cfg["NPS"], cfg["SEG"]
    KC = SEG // 128
    TPT = NPS * KC
    TST = KC
    TT = TPT + TST
    NTOK = TT * 128
    NU = NPS + 1
    NB = TT // 4
    CAPT = cfg.get("CAPT", 2)
    G = max(1, 4 // CAPT)
    while NB % G:
        G //= 2
    NS = G * CAPT * 128
    BT = []
    if NPS % 4 == 0 and cfg.get("mix", True):
        for q in range(NPS // 4):
            for j in range(KC):
                BT.append([(4 * q + s_) * KC + j for s_ in range(4)])
    else:
        for b in range(TPT // 4):
            BT.append([4 * b + t for t in range(4)])
    for b in range(TST // 4):
        BT.append([TPT + 4 * b + t for t in range(4)])
    assert len(BT) == NB
    QG = SEG // 512
    CW = 12 * SEG
    CAPS = [2 * (8 * TPT * 128) // NE, 2 * (8 * TST * 128) // NE]
    oV = 4 * SEG
    oSK = oV + KC * 512
    oSV = oSK + 2 * SEG
    NSUB = 2

    nc = bass.Bass("TRN2", target_bir_lowering=False)
    dt = nc.dram_tensor
    xp = dt("xp", [TPT * 128, D], F32, kind="ExternalInput")
    xs = dt("xs", [TST * 128, D], F32, kind="ExternalInput")
    ropet = dt("ropet", [NTOK, 128], F32, kind="ExternalInput")
    cst = dt("cst", [128, 640], F32, kind="ExternalInput")
    cct = dt("cct", [128, 16], F32, kind="ExternalInput")
    attn_norm = dt("attn_norm", [L, D], F32, kind="ExternalInput")
    w_in = dt("w_in", [L, D, INW], F32, kind="ExternalInput")
    g4 = dt("g4", [L, 256], F32, kind="ExternalInput")
    lam4 = dt("lam4", [L, 256], F32, kind="ExternalInput")
    subln = dt("subln", [L, 128], F32, kind="ExternalInput")
    sink = dt("sink", [L, 8], F32, kind="ExternalInput")
    w_out = dt("w_out", [L, D, D], F32, kind="ExternalInput")
    ffn_norm = dt("ffn_norm", [L, D], F32, kind="ExternalInput")
    w_router = dt("w_router", [L, D, NE], F32, kind="ExternalInput")
    w_gate = dt("w_gate", [L, NE, D, D], F32, kind="ExternalInput")
    w_up = dt("w_up", [L, NE, D, D], F32, kind="ExternalInput")
    w_down = dt("w_down", [L, NE, D, D], F32, kind="ExternalInput")
    yp = dt("yp", [TPT * 128, D], F32, kind="ExternalOutput")
    ys = dt("ys", [TST * 128, D], F32, kind="ExternalOutput")
    QT_d = dt("QT_d", [4, 128, NTOK], BF16, kind="Internal")
    KT_d = dt("KT_d", [4, 128, TPT * 128], BF16, kind="Internal")
    V_d = dt("V_d", [TPT * 128, 512], BF16, kind="Internal")
    SQT_d = dt("SQT_d", [8, 64, NTOK], BF16, kind="Internal")
    SKT_d = dt("SKT_d", [2, 64, TPT * 128], BF16, kind="Internal")
    SV_d = dt("SV_d", [TPT * 128, 256], BF16, kind="Internal")
    KTs_in = dt("KTs_in", [4, 128, SEG], BF16, kind="Internal")
    KTs_g = dt("KTs_g", [4, 512, SEG], BF16, kind="Internal")
    Vs_in = dt("Vs_in", [4, 128, KC * 128], BF16, kind="Internal")
    Vs_g = dt("Vs_g", [4, 512, KC * 128], BF16, kind="Internal")
    SKs_in = dt("SKs_in", [2, 64, SEG], BF16, kind="Internal")
    SKs_g = dt("SKs_g", [2, 256, SEG], BF16, kind="Internal")
    SVs_in = dt("SVs_in", [2, 128, KC * 128], BF16, kind="Internal")
    SVs_g = dt("SVs_g", [2, 512, KC * 128], BF16, kind="Internal")
    XN_d = dt("XN_d", [NTOK, D], BF16, kind="Internal")
    H_d = dt("H_d", [NTOK, D], F32, kind="Internal")
    X_d = dt("X_d", [NTOK, D], F32, kind="Internal")
    AFF_d = dt("AFF_d", [128, TT * NE], F32, kind="Internal")
    AFFG_d = dt("AFFG_d", [8 * 128, TT * NE], F32, kind="Internal")
    YE_d = dt("YE_d", [NB, NE, CAPT, 128, D], BF16, kind="Internal")

    rs = {n: Res(n) for n in ["xin", "rope", "cst", "cct", "wts", "QT_d", "KT_d", "V_d", "SQT_d", "SKT_d",
                              "SV_d", "SKV_in", "SKV_g", "XN_d", "H_d", "X_d", "AFF_d", "AFFG_d", "YE_d", "yout"]}

    def xsrc_ap(l, tile):
        if l == 0:
            return (xp.ap()[tile * 128:(tile + 1) * 128, :] if tile < TPT
                    else xs.ap()[(tile - TPT) * 128:(tile - TPT + 1) * 128, :]), rs["xin"]
        return X_d.ap()[tile * 128:(tile + 1) * 128, :], rs["X_d"]

    def xdst_ap(l, tile):
        if l == L - 1:
            return (yp.ap()[tile * 128:(tile + 1) * 128, :] if tile < TPT
                    else ys.ap()[(tile - TPT) * 128:(tile - TPT + 1) * 128, :]), rs["yout"]
        return X_d.ap()[tile * 128:(tile + 1) * 128, :], rs["X_d"]

    def bcast_rows(a):
        return AP(a.tensor, a.offset, [[0, 128]] + [list(x) for x in a.ap[1:]])

    with contextlib.ExitStack() as root:
        S = Sched(nc, root)
        op, dma = S.op, S.dma
        MUL, ADD, SUB = ALU.mult, ALU.add, ALU.subtract

        def mm(out, lhsT, rhs, start, stop, reads, writes):
            op("pe", "matmul", dict(out=out, lhsT=lhsT, rhs=rhs, start=start, stop=stop), reads, writes)

        def tr(out, in_, reads, writes):
            op("pe", "transpose", dict(out=out, in_=in_, identity=ident[:]), reads + [ident_r], writes)

        cstf, cstf_r = S.sb("cstf", [128, 640], F32)
        cc_t, cc_r = S.sb("cc_t", [128, 16], F32)
        ident, ident_r = S.sb("ident", [128, 128], BF16)
        Utri, Utri_r = S.sb("Utri", [128, 128], BF16)
        triL, triL_r = S.sb("triL", [128, 128], BF16)
        triR, triR_r = S.sb("triR", [128, 128], BF16)
        triLe, triLe_r = S.sb("triLe", [128, 128], BF16)
        triRe, triRe_r = S.sb("triRe", [128, 128], BF16)
        onesb, onesb_r = S.sb("onesb", [128, 128], BF16)
        onesf, onesf_r = S.sb("onesf", [128, 128], F32)
        AFFS, AFFS_r = S.sb("AFFS", [128, TT, NE], F32)
        posm, posm_r = S.sb("posm", [128, TT, NE], F32)
        g_attn, g_attn_r = S.sb("g_attn", [128, D], F32)
        g_ffn, g_ffn_r = S.sb("g_ffn", [128, D], F32)
        G4, G4_r = S.sb("G4", [128, 256], F32)
        L4, L4_r = S.sb("L4", [128, 256], F32)
        ltmp, ltmp_r = S.sb("ltmp", [128, 2, 64], F32)
        lsc, lsc_r = S.sb("lsc", [128, 8], F32)
        sublnS, sublnS_r = S.sb("sublnS", [128, 1], F32)
        lamneg, lamneg_r = S.sb("lamneg", [128, 1], F32)
        esink, esink_r = S.sb("esink", [128, 8], F32)
        wrf, wrf_r = S.sb("wrf", [128, 8, NE], F32)
        wrb, wrb_r = S.sb("wrb", [128, 8, NE], BF16)
        iota = cstf[:, 512:640]
        epsb, epsb_r = S.sb("epsb", [128, 1], F32)
        iota2, iota2_r = S.sb("iota2", [128, CAPT, 128], F32)
        op("dve", "memset", dict(ap=epsb[:], constant=EPS), [], [epsb_r])

        dma("sp", dict(out=cstf[:], in_=cst.ap()), cstf_r, "ld", [rs["cst"]], [cstf_r])
        dma("sp", dict(out=cc_t[:], in_=cct.ap()), cc_r, "ld", [rs["cct"]], [cc_r])
        for i, (t, r) in enumerate([(ident, ident_r), (Utri, Utri_r), (triL, triL_r), (triR, triR_r)]):
            op("dve", "tensor_copy", dict(out=t[:], in_=cstf[:, i * 128:(i + 1) * 128]), [cstf_r], [r])
        op("dve", "tensor_scalar", dict(out=triLe[:], in0=cstf[:, 256:384], scalar1=cc_t[:, 0:1], scalar2=None, op0=MUL), [cstf_r, cc_r], [triLe_r])
        op("dve", "tensor_scalar", dict(out=triRe[:], in0=cstf[:, 384:512], scalar1=cc_t[:, 1:2], scalar2=None, op0=MUL), [cstf_r, cc_r], [triRe_r])
        op("dve", "memset", dict(ap=onesb[:], constant=1.0), [], [onesb_r])
        op("dve", "memset", dict(ap=onesf[:], constant=1.0), [], [onesf_r])
        for s_ in range(CAPT):
            op("dve", "tensor_scalar", dict(out=iota2[:, s_, :], in0=iota, scalar1=float(128 * s_), scalar2=None, op0=ADD), [cstf_r], [iota2_r])

        for l in range(L):
          try:
              lam_init = 0.8 - 0.6 * math.exp(-0.3 * l)
              W = rs["wts"]
              dma("sp", dict(out=g_attn[:], in_=bcast_rows(attn_norm.ap()[l:l + 1, :])), g_attn_r, "ld", [W], [g_attn_r])
              dma("sp", dict(out=g_ffn[:], in_=bcast_rows(ffn_norm.ap()[l:l + 1, :])), g_ffn_r, "ld", [W], [g_ffn_r])
              dma("sp", dict(out=G4[:], in_=bcast_rows(g4.ap()[l:l + 1, :])), G4_r, "ld", [W], [G4_r])
              dma("sp", dict(out=L4[:], in_=bcast_rows(lam4.ap()[l:l + 1, :])), L4_r, "ld", [W], [L4_r])
              dma("sp", dict(out=esink[:], in_=bcast_rows(sink.ap()[l:l + 1, :])), esink_r, "ld", [W], [esink_r])
              dma("sp", dict(out=sublnS[:], in_=subln.ap()[l:l + 1, :].rearrange("o p -> p o")), sublnS_r, "ld", [W], [sublnS_r])
              dma("sp", dict(out=wrf[:], in_=w_router.ap()[l].rearrange("(c p) e -> p c e", p=128)), wrf_r, "ld", [W], [wrf_r])
              op("dve", "tensor_copy", dict(out=wrb[:], in_=wrf[:]), [wrf_r], [wrb_r])
              op("dve", "tensor_scalar", dict(out=sublnS[:], in0=sublnS[:], scalar1=float(1.0 - lam_init), scalar2=None, op0=MUL), [sublnS_r], [sublnS_r])
              op("act", "activation", dict(out=esink[:], in_=esink[:], func=AF.Exp), [esink_r], [esink_r])
              op("dve", "tensor_tensor", dict(out=ltmp[:], in0=V(L4[:, 0:1], [[128, 2], [1, 64]]), in1=V(L4[:, 64:65], [[128, 2], [1, 64]]), op=MUL), [L4_r], [ltmp_r])
              op("dve", "tensor_reduce", dict(out=lsc[:, 0:2], in_=ltmp[:], axis=AX.X, op=ADD), [ltmp_r], [lsc_r])
              op("act", "activation", dict(out=lsc[:, 2:4], in_=lsc[:, 0:2], func=AF.Exp), [lsc_r], [lsc_r])
              op("dve", "tensor_tensor", dict(out=lsc[:, 4:5], in0=lsc[:, 3:4], in1=lsc[:, 2:3], op=SUB), [lsc_r], [lsc_r])
              op("dve", "tensor_scalar", dict(out=lamneg[:], in0=lsc[:, 4:5], scalar1=float(-lam_init), scalar2=None, op0=ADD), [lsc_r], [lamneg_r])

              if cfg.get("stop") == "S":
                  raise StopBuild()
              with S.scope():
                  Win, Win_r = S.sb("Win", [128, 8, INW], BF16)
                  stg = [S.sb("stg%d" % i, [128, INW], F32) for i in range(2)]
                  for c in range(8):
                      st_t, st_r = stg[c % 2]
                      dma("sp", dict(out=st_t[:], in_=w_in.ap()[l, c * 128:(c + 1) * 128, :]), st_r, "ld", [W], [st_r])
                      op("pool", "tensor_copy", dict(out=Win[:, c, :], in_=st_t[:]), [st_r], [Win_r])
                  xt = [S.sb("xt%d" % i, [128, D], F32) for i in range(2)]
                  rp = [S.sb("rp%d" % i, [128, 128], F32) for i in range(2)]
                  junk, junk_r = S.sb("junk", [128, D], F32)
                  ss1, ss1_r = S.sb("ss1", [128, 4], F32)
                  xn, xn_r = S.sb("xn", [128, D], BF16)
                  xnT, xnT_r = S.sb("xnT", [128, 8, 128], BF16)
                  zq, zq_r = S.sb("zq", [128, 26, 64], F32)
                  zsq, zsq_r = S.sb("zsq", [128, 26, 64], F32)
                  hss, hss_r = S.sb("hss", [128, 26], F32)
                  hrs, hrs_r = S.sb("hrs", [128, 26], F32)
                  zn, zn_r = S.sb("zn", [128, 26, 64], F32)
                  t1, t1_r = S.sb("t1", [128, 26, 64], F32)
                  t2, t2_r = S.sb("t2", [128, 26, 64], F32)
                  zr, zr_r = S.sb("zr", [128, 26 * 64], BF16)
                  vsb = [S.sb("vsb%d" % i, [128, 512], BF16) for i in range(2)]
                  svd = [S.sb("svd%d" % i, [128, 2, 2, 64], BF16) for i in range(2)]
                  qst = [S.sb("qst%d" % i, [128, 8, NSUB * 128], BF16) for i in range(2)]
                  sst = [S.sb("sst%d" % i, [64, 10, NSUB * 128], BF16) for i in range(2)]
                  zps, zps_r = S.ps("zps", [128, 2560], F32)
                  tpb, tpb_r = S.ps("tpb", [128, 8, 128], BF16)
                  tqb, tqb_r = S.ps("tqb", [128, 8, 128], BF16)
                  tsb, tsb_r = S.ps("tsb", [64, 8, 128], BF16)

                  order = list(range(TPT, TT)) + list(range(TPT))
                  for it, tile in enumerate(order[:cfg.get("atiles", 10 ** 9)]):
                      samp = tile >= TPT
                      lt = tile - TPT if samp else tile
                      x_t, x_r = xt[it % 2]
                      r_t, r_r = rp[it % 2]
                      src, src_r = xsrc_ap(l, tile)
                      dma("sp", dict(out=x_t[:], in_=src), x_r, "ld", [src_r], [x_r])
                      dma("sp", dict(out=r_t[:], in_=ropet.ap()[tile * 128:(tile + 1) * 128, :]), r_r, "ld", [rs["rope"]], [r_r])
                      op("act", "activation", dict(out=junk[:], in_=x_t[:], func=AF.Square, accum_out=ss1[:, 0:1]), [x_r], [junk_r, ss1_r])
                      op("act", "activation", dict(out=ss1[:, 1:2], in_=ss1[:, 0:1], func=AF.Sqrt, scale=1.0 / D, bias=epsb[:, 0:1]), [ss1_r, epsb_r], [ss1_r])
                      op("dve", "reciprocal", dict(out=ss1[:, 2:3], in_=ss1[:, 1:2]), [ss1_r], [ss1_r])
                      op("dve", "scalar_tensor_tensor", dict(out=xn[:], in0=x_t[:], scalar=ss1[:, 2:3], in1=g_attn[:], op0=MUL, op1=MUL), [x_r, ss1_r, g_attn_r], [xn_r])
                      for c in range(8):
                          tr(tpb[:, c, :], xn[:, c * 128:(c + 1) * 128], [xn_r], [tpb_r])
                      op("act", "activation", dict(out=xnT[:], in_=tpb[:], func=AF.Copy), [tpb_r], [xnT_r])
                      for ng in range(5):
                          n0, n1 = ng * 512, min(INW, ng * 512 + 512)
                          for c in range(8):
                              mm(zps[:, n0:n1], xnT[:, c, :], Win[:, c, n0:n1], c == 0, c == 7, [xnT_r, Win_r], [zps_r])
                      zqf = zq[:].rearrange("p a b -> p (a b)")
                      op("act", "activation", dict(out=zqf[:, 0:1024], in_=zps[:, 0:1024], func=AF.Copy), [zps_r], [zq_r])
                      op("act", "activation", dict(out=zqf[:, 1024:1664], in_=zps[:, 1536:2176], func=AF.Copy), [zps_r], [zq_r])
                      v_t, v_r = vsb[it % 2]
                      sv_t, sv_r = svd[it % 2]
                      op("act", "activation", dict(out=v_t[:], in_=zps[:, 1024:1536], func=AF.Copy), [zps_r], [v_r])
                      for dup in range(2):
                          op("act", "activation", dict(out=sv_t[:, :, dup, :], in_=zps[:, 2176:2304].rearrange("p (h d) -> p h d", d=64), func=AF.Copy), [zps_r], [sv_r])
                      op("dve", "tensor_tensor", dict(out=zsq[:], in0=zq[:], in1=zq[:], op=MUL), [zq_r], [zsq_r])
                      op("dve", "tensor_reduce", dict(out=hss[:], in_=zsq[:], axis=AX.X, op=ADD), [zsq_r], [hss_r])
                      op("act", "activation", dict(out=hrs[:], in_=hss[:], func=AF.Sqrt, scale=1.0 / 64, bias=epsb[:, 0:1]), [hss_r, epsb_r], [hrs_r])
                      op("dve", "reciprocal", dict(out=hrs[:], in_=hrs[:]), [hrs_r], [hrs_r])
                      op("dve", "tensor_tensor", dict(out=zn[:], in0=zq[:], in1=V(hrs[:], [[1, 26], [0, 64]]), op=MUL), [zq_r, hrs_r], [zn_r])
                      for gi, (h0, h1) in enumerate([(0, 8), (8, 16), (16, 24), (24, 26)]):
                          op("pool", "tensor_tensor", dict(out=zn[:, h0:h1, :], in0=zn[:, h0:h1, :], in1=V(G4[:, gi * 64:gi * 64 + 1], [[0, h1 - h0], [1, 64]]), op=MUL), [zn_r, G4_r], [zn_r])
                      op("pool", "tensor_tensor", dict(out=t1[:], in0=zn[:], in1=V(r_t[:, 0:1], [[0, 26], [1, 64]]), op=MUL), [zn_r, r_r], [t1_r])
                      op("pool", "tensor_tensor", dict(out=t2[:, :, 0:32], in0=zn[:, :, 32:64], in1=V(r_t[:, 64:65], [[0, 26], [1, 32]]), op=MUL), [zn_r, r_r], [t2_r])
                      op("pool", "tensor_tensor", dict(out=t2[:, :, 32:64], in0=zn[:, :, 0:32], in1=V(r_t[:, 96:97], [[0, 26], [1, 32]]), op=MUL), [zn_r, r_r], [t2_r])
                      op("dve", "tensor_tensor", dict(out=zr[:].rearrange("p (a b) -> p a b", b=64), in0=t1[:], in1=t2[:], op=ADD), [t1_r, t2_r], [zr_r])
                      sub = it % NSUB
                      q_t, q_r = qst[(it // NSUB) % 2]
                      s_t, s_r = sst[(it // NSUB) % 2]
                      for j in range(8):
                          tr(tqb[:, j, :], zr[:, j * 128:(j + 1) * 128], [zr_r], [tqb_r])
                      op("act", "activation", dict(out=q_t[:, :, sub * 128:(sub + 1) * 128], in_=tqb[:], func=AF.Copy), [tqb_r], [q_r])
                      for j in range(8):
                          tr(tsb[:, j, :], zr[:, 1024 + j * 64:1024 + (j + 1) * 64], [zr_r], [tsb_r])
                      op("dve", "tensor_copy", dict(out=s_t[:, 0:8, sub * 128:(sub + 1) * 128], in_=tsb[:]), [tsb_r], [s_r])
                      for j in range(2):
                          tr(tsb[:, j, :], zr[:, 1536 + j * 64:1536 + (j + 1) * 64], [zr_r], [tsb_r])
                      op("dve", "tensor_copy", dict(out=s_t[:, 8:10, sub * 128:(sub + 1) * 128], in_=tsb[:, 0:2, :]), [tsb_r], [s_r])
                      if samp:
                          dma("act", dict(out=Vs_in.ap()[:, :, lt * 128:(lt + 1) * 128].rearrange("h p e -> p h e"), in_=v_t[:].rearrange("p (h e) -> p h e", h=4)), v_r, "st", [v_r], [rs["SKV_in"]], merge=True)
                          dma("act", dict(out=SVs_in.ap()[:, :, lt * 128:(lt + 1) * 128].rearrange("h p e -> p h e"), in_=sv_t[:].rearrange("p a b c -> p a (b c)")), sv_r, "st", [sv_r], [rs["SKV_in"]], merge=True)
                      else:
                          dma("act", dict(out=V_d.ap()[lt * 128:(lt + 1) * 128, :], in_=v_t[:]), v_r, "st", [v_r], [rs["V_d"]], merge=True)
                          dma("act", dict(out=SV_d.ap()[lt * 128:(lt + 1) * 128, :], in_=sv_t[:].rearrange("p a b c -> p (a b c)")), sv_r, "st", [sv_r], [rs["SV_d"]], merge=True)
                      if sub == NSUB - 1:
                          tk0 = (tile - (NSUB - 1)) * 128
                          lk0 = (lt - (NSUB - 1)) * 128
                          nt = NSUB * 128
                          dma("sp", dict(out=QT_d.ap()[:, :, tk0:tk0 + nt].rearrange("h p t -> p h t"), in_=q_t[:, 0:4, :]), q_r, "st", [q_r], [rs["QT_d"]], merge=True)
                          dma("sp", dict(out=SQT_d.ap()[:, :, tk0:tk0 + nt].rearrange("h p t -> p h t"), in_=s_t[:, 0:8, :]), s_r, "st", [s_r], [rs["SQT_d"]], merge=True)
                          if samp:
                              dma("sp", dict(out=KTs_in.ap()[:, :, lk0:lk0 + nt].rearrange("h p t -> p h t"), in_=q_t[:, 4:8, :]), q_r, "st", [q_r], [rs["SKV_in"]], merge=True)
                              dma("sp", dict(out=SKs_in.ap()[:, :, lk0:lk0 + nt].rearrange("h p t -> p h t"), in_=s_t[:, 8:10, :]), s_r, "st", [s_r], [rs["SKV_in"]], merge=True)
                          else:
                              dma("sp", dict(out=KT_d.ap()[:, :, lk0:lk0 + nt].rearrange("h p t -> p h t"), in_=q_t[:, 4:8, :]), q_r, "st", [q_r], [rs["KT_d"]], merge=True)
                              dma("sp", dict(out=SKT_d.ap()[:, :, lk0:lk0 + nt].rearrange("h p t -> p h t"), in_=s_t[:, 8:10, :]), s_r, "st", [s_r], [rs["SKT_d"]], merge=True)
                      if it == TST - 1 and not cfg.get("nocc"):
                          for (ti, tg, nh) in ((KTs_in, KTs_g, 4), (Vs_in, Vs_g, 4), (SKs_in, SKs_g, 2), (SVs_in, SVs_g, 2)):
                              for hh in range(nh):
                                  S.custom("pool", "collective_compute", dict(kind="AllGather", op=ALU.bypass, replica_groups=[[0, 1, 2, 3], [4, 5, 6, 7]],
                                                                              ins=[ti.ap()[hh]], outs=[tg.ap()[hh]]), "CCKV", 1, [rs["SKV_in"]], [rs["SKV_g"]])
              if cfg.get("stop") == "A":
                  raise StopBuild()
              with S.scope():
                  Wout, Wout_r = S.sb("Wout", [128, 8, D], BF16)
                  stg = [S.sb("stgo%d" % i, [128, D], F32) for i in range(2)]
                  for c in range(8):
                      st_t, st_r = stg[c % 2]
                      dma("sp", dict(out=st_t[:], in_=w_out.ap()[l, c * 128:(c + 1) * 128, :]), st_r, "ld", [W], [st_r])
                      op("pool", "tensor_copy", dict(out=Wout[:, c, :], in_=st_t[:]), [st_r], [Wout_r])
                  mT, mT_r = S.sb("mT", [128, 8, SEG], BF16)
                  KTs = [S.sb("KTs%d" % i, [128, SEG], BF16) for i in range(2)]
                  Vs = [S.sb("Vs%d" % i, [128, KC, 128], BF16) for i in range(2)]
                  QTs = [S.sb("QTs%d" % i, [128, 512], BF16) for i in range(2)]
                  PTs = [S.sb("PTs%d" % i, [128, 512], BF16) for i in range(4)]
                  rinv = [S.sb("rinv%d" % i, [128, 512], F32) for i in range(2)]
                  av = [S.sb("av%d" % i, [128, 512], F32) for i in range(2)]
                  asq, asq_r = S.sb("asq", [128, 512], BF16)
                  accP = [S.sb("accP%d" % i, [128, 512], F32) for i in range(2)]
                  accB = [S.sb("accB%d" % i, [128, 512], BF16) for i in range(2)]
                  rrs, rrs_r = S.sb("rrs", [128, 512], F32)
                  SKe, SKe_r = S.sb("SKe", [64, 2, SEG + 256], BF16)
                  SVe, SVe_r = S.sb("SVe", [128, KC + 2, 2, 128], BF16)
                  hK, hK_r = S.sb("hK", [64, 2, 4, 2, 128], BF16)
                  hV, hV_r = S.sb("hV", [128, 2, 4, 256], BF16)
                  SQs = [S.sb("SQs%d" % i, [64, 8, 512], BF16) for i in range(2)]
                  Rp, Rp_r = S.sb("Rp", [128, 512], F32)
                  xt2 = [S.sb("xd%d" % i, [128, D], F32) for i in range(2)]
                  ht = [S.sb("ht%d" % i, [128, D], F32) for i in range(2)]
                  junk2, junk2_r = S.sb("junk2", [128, D], F32)
                  ss2, ss2_r = S.sb("ss2", [128, 8], F32)
                  n2 = [S.sb("n2%d" % i, [128, D], BF16) for i in range(2)]
                  n2T, n2T_r = S.sb("n2T", [128, 8, 128], BF16)
                  ex, ex_r = S.sb("ex", [128, NE], F32)
                  ST, ST_r = S.ps("ST", [128, 2, 512], F32)
                  STr = [Res("ST0"), Res("ST1")]
                  OT, OT_r = S.ps("OT", [128, 2, 512], F32)
                  OTr = [Res("OT0"), Res("OT1")]
                  RT, RT_r = S.ps("RT", [128, 2, 512], F32)
                  RTr = [Res("RT0"), Res("RT1")]
                  XF, XF_r = S.ps("XF", [128, 512], F32)
                  XB, XB_r = S.ps("XB", [128, 8, 128], BF16)
                  pcnt = 0
                  segc = 0
                  qgc = 0
                  for u in range(NU):
                      samp = u == NU - 1
                      t0 = u * SEG
                      nseg = 4 if samp else 1
                      for h in range(4):
                          for qg in range(QG):
                              Q_t, Q_r = QTs[qgc % 2]
                              qgc += 1
                              dma("sp", dict(out=Q_t[:], in_=QT_d.ap()[h, :, t0 + qg * 512: t0 + (qg + 1) * 512]), Q_r, "ld", [rs["QT_d"]], [Q_r])
                              for sg in range(nseg):
                                  K_t, K_r = KTs[segc % 2]
                                  V_t, V_r = Vs[segc % 2]
                                  segc += 1
                                  if samp:
                                      ksrc = KTs_g.ap()[h, sg * 128:(sg + 1) * 128, :]
                                      vsrc = Vs_g.ap()[h, sg * 128:(sg + 1) * 128, :].rearrange("p (c e) -> p c e", e=128)
                                      kr, vr = rs["SKV_g"], rs["SKV_g"]
                                  else:
                                      ksrc = KT_d.ap()[h, :, t0:t0 + SEG]
                                      vsrc = V_d.ap()[t0:t0 + SEG, h * 128:(h + 1) * 128].rearrange("(c p) e -> p c e", p=128)
                                      kr, vr = rs["KT_d"], rs["V_d"]
                                  dma("sp", dict(out=K_t[:], in_=ksrc), K_r, "ld", [kr], [K_r])
                                  dma("sp", dict(out=V_t[:], in_=vsrc), V_r, "ld", [vr], [V_r])
                                  for kc in range(KC):
                                      first = sg == 0 and kc == 0
                                      last = sg == nseg - 1 and kc == KC - 1
                                      for c in range(2):
                                          mm(ST[:, c, :], K_t[c * 64:(c + 1) * 64, kc * 128:(kc + 1) * 128], Q_t[c * 64:(c + 1) * 64, :], True, True, [K_r, Q_r], [STr[c]])
                                          P_t, P_r = PTs[pcnt % 4]
                                          pcnt += 1
                                          op("act", "activation", dict(out=P_t[:], in_=ST[:, c, :], func=AF.Exp, scale=0.125), [STr[c]], [P_r])
                                          mm(OT[:, c, :], V_t[:, kc, :], P_t[:], first, last, [V_r, P_r], [OTr[c]])
                                          if first:
                                              op("pool", "tensor_copy", dict(out=accP[c][0][:], in_=P_t[:]), [P_r], [accP[c][1]])
                                          else:
                                              op("pool", "tensor_tensor", dict(out=accP[c][0][:], in0=accP[c][0][:], in1=P_t[:], op=ADD), [P_r, accP[c][1]], [accP[c][1]])
                              for c in range(2):
                                  op("pool", "tensor_copy", dict(out=accB[c][0][:], in_=accP[c][0][:]), [accP[c][1]], [accB[c][1]])
                                  mm(RT[:, c, :], onesb[:], accB[c][0][:], True, True, [onesb_r, accB[c][1]], [RTr[c]])
                              for c in range(2):
                                  ri_t, ri_r = rinv[c]
                                  op("dve", "reciprocal", dict(out=ri_t[:], in_=RT[:, c, :]), [RTr[c]], [ri_r])
                                  a_t, a_r = av[c]
                                  op("dve", "tensor_tensor", dict(out=a_t[:], in0=OT[:, c, :], in1=ri_t[:], op=MUL), [OTr[c], ri_r], [a_r])
                              op("dve", "scalar_tensor_tensor", dict(out=av[0][0][:], in0=av[1][0][:], scalar=lamneg[:, 0:1], in1=av[0][0][:], op0=MUL, op1=ADD), [av[0][1], av[1][1], lamneg_r], [av[0][1]])
                              op("act", "activation", dict(out=asq[:], in_=av[0][0][:], func=AF.Square), [av[0][1]], [asq_r])
                              mm(XF[:], onesb[:], asq[:], True, True, [onesb_r, asq_r], [XF_r])
                              op("act", "activation", dict(out=rrs[:], in_=XF[:], func=AF.Sqrt, scale=1.0 / 128, bias=epsb[:, 0:1]), [XF_r, epsb_r], [rrs_r])
                              op("dve", "reciprocal", dict(out=rrs[:], in_=rrs[:]), [rrs_r], [rrs_r])
                              op("dve", "scalar_tensor_tensor", dict(out=mT[:, h, qg * 512:(qg + 1) * 512], in0=av[0][0][:], scalar=sublnS[:, 0:1], in1=rrs[:], op0=MUL, op1=MUL), [av[0][1], sublnS_r, rrs_r], [mT_r])
                      if samp:
                          dma("sp", dict(out=SKe[:, :, 128:128 + SEG], in_=SKs_in.ap().rearrange("h p t -> p h t")), SKe_r, "ld", [rs["SKV_in"]], [SKe_r])
                          for hh in range(2):
                              dma("sp", dict(out=SVe[:, 1:KC + 1, hh, :], in_=SVs_in.ap()[hh].rearrange("p (c e) -> p c e", e=128)), SVe_r, "ld", [rs["SKV_in"]], [SVe_r])
                          for r in range(4):
                              gkr = SKs_g.ap()[:, r * 64:(r + 1) * 64, :].rearrange("h p t -> p h t")
                              dma("sp", dict(out=hK[:, 0, r], in_=gkr[:, :, SEG - 128:SEG]), hK_r, "ld", [rs["SKV_g"]], [hK_r])
                              dma("sp", dict(out=hK[:, 1, r], in_=gkr[:, :, 0:128]), hK_r, "ld", [rs["SKV_g"]], [hK_r])
                          for hh in range(2):
                              gv = SVs_g.ap()[hh].rearrange("(r p) c -> p r c", p=128)
                              dma("sp", dict(out=hV[:, 0, :, hh * 128:(hh + 1) * 128], in_=gv[:, :, (KC - 1) * 128:KC * 128]), hV_r, "ld", [rs["SKV_g"]], [hV_r])
                              dma("sp", dict(out=hV[:, 1, :, hh * 128:(hh + 1) * 128], in_=gv[:, :, 0:128]), hV_r, "ld", [rs["SKV_g"]], [hV_r])
                          for side in range(2):
                              kdst = SKe[:, :, 0:128] if side == 0 else SKe[:, :, 128 + SEG:256 + SEG]
                              vdst = (SVe[:, 0, :, :] if side == 0 else SVe[:, KC + 1, :, :]).rearrange("p h d -> p (h d)")
                              for r in range(4):
                                  wcol = cc_t[:, 2 + side * 4 + r: 3 + side * 4 + r]
                                  if r == 0:
                                      op("dve", "tensor_scalar", dict(out=kdst, in0=hK[:, side, r], scalar1=wcol[0:64, :], scalar2=None, op0=MUL), [hK_r, cc_r], [SKe_r])
                                      op("dve", "tensor_scalar", dict(out=vdst, in0=hV[:, side, r], scalar1=wcol, scalar2=None, op0=MUL), [hV_r, cc_r], [SVe_r])
                                  else:
                                      op("dve", "scalar_tensor_tensor", dict(out=kdst, in0=hK[:, side, r], scalar=wcol[0:64, :], in1=kdst, op0=MUL, op1=ADD), [hK_r, cc_r, SKe_r], [SKe_r])
                                      op("dve", "scalar_tensor_tensor", dict(out=vdst, in0=hV[:, side, r], scalar=wcol, in1=vdst, op0=MUL, op1=ADD), [hV_r, cc_r, SVe_r], [SVe_r])
                      else:
                          dma("sp", dict(out=SKe[:, :, 128:128 + SEG], in_=SKT_d.ap()[:, :, t0:t0 + SEG].rearrange("h p t -> p h t")), SKe_r, "ld", [rs["SKT_d"]], [SKe_r])
                          dma("sp", dict(out=SVe[:, 1:KC + 1, :, :].rearrange("p c h d -> p c (h d)"), in_=SV_d.ap()[t0:t0 + SEG, :].rearrange("(c p) e -> p c e", p=128)), SVe_r, "ld", [rs["SV_d"]], [SVe_r])
                      for n in range(KC):
                          if n % 4 == 0:
                              SQ_t, SQ_r = SQs[(n // 4) % 2]
                              dma("sp", dict(out=SQ_t[:], in_=SQT_d.ap()[:, :, t0 + n * 128: t0 + n * 128 + 512].rearrange("h p t -> p h t")), SQ_r, "ld", [rs["SQT_d"]], [SQ_r])
                          for h in range(2):
                              js = [j for j in range(3) if samp or (0 < n + j < KC + 1)]
                              for ji, j in enumerate(js):
                                  ci = n + j
                                  c = pcnt % 2
                                  mm(ST[:, c, :], SKe[:, h, ci * 128:(ci + 1) * 128], SQ_t[:, h * 4:(h + 1) * 4, (n % 4) * 128:(n % 4 + 1) * 128], True, True, [SKe_r, SQ_r], [STr[c]])
                                  P_t, P_r = PTs[pcnt % 4]
                                  pcnt += 1
                                  op("act", "activation", dict(out=P_t[:], in_=ST[:, c, :], func=AF.Exp, scale=0.125), [STr[c]], [P_r])
                                  if j != 1:
                                      if j == 0:
                                          mk_t, mk_r = (triLe, triLe_r) if (samp and n == 0) else (triL, triL_r)
                                      else:
                                          mk_t, mk_r = (triRe, triRe_r) if (samp and n == KC - 1) else (triR, triR_r)
                                      op("dve", "tensor_tensor", dict(out=P_t[:].rearrange("p (g q) -> p g q", g=4), in0=P_t[:].rearrange("p (g q) -> p g q", g=4), in1=V(mk_t[:, 0:1], [[0, 4], [1, 128]]), op=MUL), [P_r, mk_r], [P_r])
                                  mm(OT[:, 0, :], SVe[:, ci, h, :], P_t[:], ji == 0, ji == len(js) - 1, [SVe_r, P_r], [OTr[0]])
                                  mm(RT[:, 0, :], onesb[:], P_t[:], ji == 0, ji == len(js) - 1, [onesb_r, P_r], [RTr[0]])
                              op("dve", "tensor_tensor", dict(out=Rp[:].rearrange("p (g q) -> p g q", g=4), in0=RT[:, 0, :].rearrange("p (g q) -> p g q", g=4), in1=V(esink[:, h * 4:h * 4 + 1], [[1, 4], [0, 128]]), op=ADD), [RTr[0], esink_r], [Rp_r])
                              op("dve", "reciprocal", dict(out=Rp[:], in_=Rp[:]), [Rp_r], [Rp_r])
                              for pp in range(2):
                                  o_in = V(OT[pp * 64:(pp + 1) * 64, 0, pp * 128:pp * 128 + 1], [[256, 2], [1, 128]])
                                  r_in = V(Rp[pp * 64:(pp + 1) * 64, pp * 128:pp * 128 + 1], [[256, 2], [1, 128]])
                                  m_out = mT[pp * 64:(pp + 1) * 64, 4 + h * 2:4 + h * 2 + 2, n * 128:(n + 1) * 128]
                                  op("dve", "tensor_tensor", dict(out=m_out, in0=o_in, in1=r_in, op=MUL), [OTr[0], Rp_r], [mT_r])
                      for n in range(KC):
                          tile = u * KC + n
                          x_t, x_r = xt2[n % 2]
                          h_t, h_r = ht[n % 2]
                          n_t, n_r = n2[n % 2]
                          src, src_r = xsrc_ap(l, tile)
                          dma("sp", dict(out=x_t[:], in_=src), x_r, "ld", [src_r], [x_r])
                          for half in range(2):
                              for c in range(8):
                                  mm(OT[:, half, :], mT[:, c, n * 128:(n + 1) * 128], Wout[:, c, half * 512:(half + 1) * 512], c == 0, c == 7, [mT_r, Wout_r], [OTr[half]])
                          op("dve", "tensor_tensor", dict(out=h_t[:].rearrange("p (a b) -> p a b", a=2), in0=OT[:], in1=x_t[:].rearrange("p (a b) -> p a b", a=2), op=ADD), [OTr[0], OTr[1], x_r], [h_r])
                          dma("act", dict(out=H_d.ap()[tile * 128:(tile + 1) * 128, :], in_=h_t[:]), h_r, "st", [h_r], [rs["H_d"]], merge=True)
                          op("act", "activation", dict(out=junk2[:], in_=h_t[:], func=AF.Square, accum_out=ss2[:, 0:1]), [h_r], [junk2_r, ss2_r])
                          op("act", "activation", dict(out=ss2[:, 1:2], in_=ss2[:, 0:1], func=AF.Sqrt, scale=1.0 / D, bias=epsb[:, 0:1]), [ss2_r, epsb_r], [ss2_r])
                          op("dve", "reciprocal", dict(out=ss2[:, 2:3], in_=ss2[:, 1:2]), [ss2_r], [ss2_r])
                          op("dve", "scalar_tensor_tensor", dict(out=n_t[:], in0=h_t[:], scalar=ss2[:, 2:3], in1=g_ffn[:], op0=MUL, op1=MUL), [h_r, ss2_r, g_ffn_r], [n_r])
                          dma("act", dict(out=XN_d.ap()[tile * 128:(tile + 1) * 128, :], in_=n_t[:]), n_r, "st", [n_r], [rs["XN_d"]], merge=True)
                          for c in range(8):
                              tr(XB[:, c, :], n_t[:, c * 128:(c + 1) * 128], [n_r], [XB_r])
                          op("act", "activation", dict(out=n2T[:], in_=XB[:], func=AF.Copy), [XB_r], [n2T_r])
                          for c in range(8):
                              mm(XF[:, 0:NE], n2T[:, c, :], wrb[:, c, :], c == 0, c == 7, [n2T_r, wrb_r], [XF_r])
                          op("dve", "tensor_reduce", dict(out=ss2[:, 3:4], in_=XF[:, 0:NE], axis=AX.X, op=ALU.max), [XF_r], [ss2_r])
                          op("dve", "tensor_scalar", dict(out=ss2[:, 4:5], in0=ss2[:, 3:4], scalar1=-1.0, scalar2=None, op0=MUL), [ss2_r], [ss2_r])
                          op("act", "activation", dict(out=ex[:], in_=XF[:, 0:NE], func=AF.Exp, bias=ss2[:, 4:5], accum_out=ss2[:, 5:6]), [XF_r, ss2_r], [ex_r, ss2_r])
                          op("dve", "reciprocal", dict(out=ss2[:, 6:7], in_=ss2[:, 5:6]), [ss2_r], [ss2_r])
                          op("dve", "tensor_scalar", dict(out=AFFS[:, tile, :], in0=ex[:], scalar1=ss2[:, 6:7], scalar2=None, op0=MUL), [ex_r, ss2_r], [AFFS_r])
              if cfg.get("stop") == "BD":
                  raise StopBuild()
              with S.scope():
                  dma("sp", dict(out=AFF_d.ap(), in_=AFFS[:].rearrange("p t e -> p (t e)")), AFFS_r, "st", [AFFS_r], [rs["AFF_d"]])
                  S.custom("pool", "collective_compute", dict(kind="AllGather", op=ALU.bypass, replica_groups=[[0, 1, 2, 3, 4, 5, 6, 7]],
                                                              ins=[AFF_d.ap()], outs=[AFFG_d.ap()]), "CCAFF", 1, [rs["AFF_d"]], [rs["AFFG_d"]])
                  NTmax = max(TPT, TST)
                  Ag, Ag_r = S.sb("Ag", [128, 8, NTmax * NE], F32)
                  cmpb, cmpb_r = S.sb("cmpb", [128, 8 * NTmax * NE], BF16)
                  cntp, cntp_r = S.sb("cntp", [128, 2, NE], BF16)
                  cntf, cntf_r = S.sb("cntf", [128, 2, NE], F32)
                  lo, lo_r = S.sb("lo", [128, NE], F32)
                  hi, hi_r = S.sb("hi", [128, NE], F32)
                  mid, mid_r = S.sb("mid", [128, NE], F32)
                  ge, ge_r = S.sb("ge", [128, NE], F32)
                  dd, dd_r = S.sb("dd", [128, NE], F32)
                  thr = [S.sb("thr%d" % i, [128, NE], F32) for i in range(2)]
                  maskb, maskb_r = S.sb("maskb", [128, TT, NE], BF16)
                  pcn, pcn_r = S.ps("pcn", [128, NE], F32)
                  cps, cps_r = S.ps("cps", [128, TT * NE], F32)
                  for grp in range(2):
                      tl0, nt = (0, TPT) if grp == 0 else (TPT, TST)
                      n_el = 8 * nt
                      dma("sp", dict(out=Ag[:, :, 0:nt * NE], in_=AFFG_d.ap().rearrange("(r p) c -> p r c", p=128)[:, :, tl0 * NE:(tl0 + nt) * NE]), Ag_r, "ld", [rs["AFFG_d"]], [Ag_r])
                      op("dve", "memset", dict(ap=lo[:], constant=0.0), [], [lo_r])
                      op("dve", "memset", dict(ap=hi[:], constant=1.0), [], [hi_r])
                      a_in = V(Ag[:, 0, 0:1], [[NTmax * NE, 8], [NE, nt], [1, NE]])
                      c_out = V(cmpb[:, 0:1], [[nt * NE, 8], [NE, nt], [1, NE]])
                      c_red = V(cmpb[:, 0:1], [[4 * nt * NE, 2], [1, NE], [NE, 4 * nt]])
                      for itn in range(NITER):
                          op("dve", "tensor_tensor", dict(out=mid[:], in0=lo[:], in1=hi[:], op=ADD), [lo_r, hi_r], [mid_r])
                          op("dve", "tensor_scalar", dict(out=mid[:], in0=mid[:], scalar1=0.5, scalar2=None, op0=MUL), [mid_r], [mid_r])
                          op("dve", "tensor_tensor", dict(out=c_out, in0=a_in, in1=V(mid[:, 0:1], [[0, 8], [0, nt], [1, NE]]), op=ALU.is_ge), [Ag_r, mid_r], [cmpb_r])
                          op("dve", "tensor_reduce", dict(out=cntf[:], in_=c_red, axis=AX.X, op=ADD), [cmpb_r], [cntf_r])
                          op("dve", "tensor_copy", dict(out=cntp[:], in_=cntf[:]), [cntf_r], [cntp_r])
                          mm(pcn[:], onesb[:], cntp[:, 0, :], True, False, [onesb_r, cntp_r], [pcn_r])
                          mm(pcn[:], onesb[:], cntp[:, 1, :], False, True, [onesb_r, cntp_r], [pcn_r])
                          op("dve", "tensor_scalar", dict(out=ge[:], in0=pcn[:], scalar1=float(CAPS[grp]) - 0.5, scalar2=None, op0=ALU.is_ge), [pcn_r], [ge_r])
                          op("dve", "tensor_tensor", dict(out=dd[:], in0=mid[:], in1=lo[:], op=SUB), [mid_r, lo_r], [dd_r])
                          op("dve", "tensor_tensor", dict(out=dd[:], in0=dd[:], in1=ge[:], op=MUL), [dd_r, ge_r], [dd_r])
                          op("dve", "tensor_tensor", dict(out=lo[:], in0=lo[:], in1=dd[:], op=ADD), [lo_r, dd_r], [lo_r])
                          op("dve", "tensor_tensor", dict(out=dd[:], in0=hi[:], in1=mid[:], op=SUB), [mid_r, hi_r], [dd_r])
                          op("dve", "tensor_tensor", dict(out=dd[:], in0=dd[:], in1=ge[:], op=MUL), [dd_r, ge_r], [dd_r])
                          op("dve", "tensor_tensor", dict(out=hi[:], in0=mid[:], in1=dd[:], op=ADD), [mid_r, dd_r], [hi_r])
                      op("dve", "tensor_copy", dict(out=thr[grp][0][:], in_=lo[:]), [lo_r], [thr[grp][1]])
                      op("dve", "tensor_tensor", dict(out=maskb[:, tl0:tl0 + nt, :], in0=AFFS[:, tl0:tl0 + nt, :], in1=V(thr[grp][0][:, 0:1], [[0, nt], [1, NE]]), op=ALU.is_ge), [AFFS_r, thr[grp][1]], [maskb_r])
                  for b in range(NB):
                      for j, t in enumerate(BT[b]):
                          mm(cps[:, t * NE:(t + 1) * NE], Utri[:], maskb[:, t, :], True, j == 0, [Utri_r, maskb_r], [cps_r])
                          for jj in range(j):
                              mm(cps[:, t * NE:(t + 1) * NE], onesb[:], maskb[:, BT[b][jj], :], False, jj == j - 1, [onesb_r, maskb_r], [cps_r])
                  op("dve", "tensor_tensor", dict(out=posm[:].rearrange("p t e -> p (t e)"), in0=cps[:], in1=maskb[:].rearrange("p t e -> p (t e)"), op=MUL), [cps_r, maskb_r], [posm_r])
                  op("dve", "tensor_scalar", dict(out=posm[:], in0=posm[:], scalar1=-1.0, scalar2=None, op0=ADD), [posm_r], [posm_r])

              if cfg.get("stop") == "E":
                  raise StopBuild()
              with S.scope():
                  Wg = [S.sb("Wg%d" % i, [128, 8, D], BF16) for i in range(2)]
                  Wu = [S.sb("Wu%d" % i, [128, 8, D], BF16) for i in range(2)]
                  Wd = [S.sb("Wd%d" % i, [128, 8, D], BF16) for i in range(2)]
                  stg = [S.sb("stgf%d" % i, [128, D], F32) for i in range(4)]
                  XNb = [S.sb("XNb%d" % i, [128, 4, D], BF16) for i in range(2)]
                  sel = [S.sb("sel%d" % i, [128, CAPT, 4, 128], BF16) for i in range(2)]
                  XET = [S.sb("XET%d" % i, [128, 8, NS], BF16) for i in range(2)]
                  hT, hT_r = S.sb("hT", [128, 8, NS], BF16)
                  sg_ = [S.sb("sg%d" % i, [128, NS], F32) for i in range(2)]
                  ye = [S.sb("ye%d" % i, [128, D], BF16) for i in range(2)]
                  gps, gps_r = S.ps("gps", [128, 8, 128], F32)
                  gu = [S.ps("gu%d" % i, [128, 2, 512], F32) for i in range(2)]
                  yps, yps_r = S.ps("yps", [128, 2, 512], F32)
                  stc = 0
                  bcn = 0
                  gcn = 0
                  fcn = 0
                  ycn = 0

                  def load_w(e):
                      nonlocal stc
                      for (wsrc, wdst) in ((w_gate, Wg), (w_up, Wu), (w_down, Wd)):
                          w_t, w_r = wdst[e % 2]
                          for c in range(8):
                              st_t, st_r = stg[stc % 4]
                              stc += 1
                              dma("sp", dict(out=st_t[:], in_=wsrc.ap()[l, e, c * 128:(c + 1) * 128, :]), st_r, "ld", [W], [st_r])
                              op("pool", "tensor_copy", dict(out=w_t[:, c, :], in_=st_t[:]), [st_r], [w_r])
                  load_w(0)
                  for e in range(NE):
                      if e + 1 < NE:
                          load_w(e + 1)
                      Wg_t, Wg_r = Wg[e % 2]
                      Wu_t, Wu_r = Wu[e % 2]
                      Wd_t, Wd_r = Wd[e % 2]
                      for gi in range(NB // G):
                          X_t, X_r = XET[gcn % 2]
                          gcn += 1
                          for bi in range(G):
                              b = gi * G + bi
                              B_t, B_r = XNb[bcn % 2]
                              s_t, s_r = sel[bcn % 2]
                              bcn += 1
                              for t in range(4):
                                  tl = BT[b][t]
                                  dma("act", dict(out=B_t[:, t, :], in_=XN_d.ap()[tl * 128:(tl + 1) * 128, :]), B_r, "ld", [rs["XN_d"]], [B_r])
                              for s_ in range(CAPT):
                                  for t in range(4):
                                      op("dve", "tensor_scalar", dict(out=s_t[:, s_, t, :], in0=iota2[:, s_, :], scalar1=posm[:, BT[b][t], e:e + 1], scalar2=None, op0=ALU.is_equal), [iota2_r, posm_r], [s_r])
                              for s_ in range(CAPT):
                                  for c in range(8):
                                      for t in range(4):
                                          mm(gps[:, c, :], B_t[:, t, c * 128:(c + 1) * 128], s_t[:, s_, t, :], t == 0, t == 3, [B_r, s_r], [gps_r])
                                  so = (bi * CAPT + s_) * 128
                                  if s_ % 2 == 0:
                                      op("act", "activation", dict(out=X_t[:, :, so:so + 128], in_=gps[:], func=AF.Copy), [gps_r], [X_r])
                                  else:
                                      op("dve", "tensor_copy", dict(out=X_t[:, :, so:so + 128], in_=gps[:]), [gps_r], [X_r])
                          for fc in range(8):
                              gu_t, gu_r = gu[fcn % 2]
                              s_t2, s_r2 = sg_[fcn % 2]
                              fcn += 1
                              for c in range(8):
                                  mm(gu_t[:, 0, 0:NS], Wg_t[:, c, fc * 128:(fc + 1) * 128], X_t[:, c, :], c == 0, c == 7, [Wg_r, X_r], [gu_r])
                              for c in range(8):
                                  mm(gu_t[:, 1, 0:NS], Wu_t[:, c, fc * 128:(fc + 1) * 128], X_t[:, c, :], c == 0, c == 7, [Wu_r, X_r], [gu_r])
                              op("act", "activation", dict(out=s_t2[:], in_=gu_t[:, 0, 0:NS], func=AF.Silu), [gu_r], [s_r2])
                              op("dve", "tensor_tensor", dict(out=hT[:, fc, :], in0=gu_t[:, 1, 0:NS], in1=s_t2[:], op=MUL), [gu_r, s_r2], [hT_r])
                          for sti in range(G * CAPT):
                              b = gi * G + sti // CAPT
                              s_ = sti % CAPT
                              for half in range(2):
                                  for fc in range(8):
                                      mm(yps[:, half, :], hT[:, fc, sti * 128:(sti + 1) * 128], Wd_t[:, fc, half * 512:(half + 1) * 512], fc == 0, fc == 7, [hT_r, Wd_r], [yps_r])
                              y_t, y_r = ye[ycn % 2]
                              ycn += 1
                              if ycn % 2:
                                  op("act", "activation", dict(out=y_t[:].rearrange("p (a b) -> p a b", a=2), in_=yps[:], func=AF.Copy), [yps_r], [y_r])
                              else:
                                  op("dve", "tensor_copy", dict(out=y_t[:].rearrange("p (a b) -> p a b", a=2), in_=yps[:]), [yps_r], [y_r])
                              dma("sp", dict(out=YE_d.ap()[b, e, s_], in_=y_t[:]), y_r, "st", [y_r], [rs["YE_d"]], merge=True)

              if cfg.get("stop") == "F1":
                  raise StopBuild()
              with S.scope():
                  NES = NE * CAPT
                  YEb = [S.sb("YEb%d" % i, [128, NES, D], BF16) for i in range(2)]
                  selg = [S.sb("selg%d" % i, [128, NE, CAPT * 128], BF16) for i in range(1)]
                  selT = [S.sb("selT%d" % i, [128, NES, 128], BF16) for i in range(2)]
                  hin = [S.sb("hin%d" % i, [128, D], F32) for i in range(2)]
                  xo = [S.sb("xo%d" % i, [128, D], F32) for i in range(2)]
                  sps, sps_r = S.ps("sps", [128, NES, 128], BF16)
                  yo = [S.ps("yo%d" % i, [128, 2, 512], F32) for i in range(2)]
                  for b in range(NB):
                      Y_t, Y_r = YEb[b % 2]
                      for e4 in range(4):
                          dma("act", dict(out=Y_t[:, e4 * 4 * CAPT:(e4 + 1) * 4 * CAPT, :], in_=YE_d.ap()[b, e4 * 4:(e4 + 1) * 4].rearrange("e s p d -> p (e s) d")), Y_r, "ld", [rs["YE_d"]], [Y_r])
                      for t in range(4):
                          tile = BT[b][t]
                          k = t % 2
                          sg_t, sg_r = selg[0]
                          sT_t, sT_r = selT[k]
                          h_t, h_r = hin[k]
                          o_t, o_r = xo[k]
                          yo_t, yo_r = yo[k]
                          dma("sp", dict(out=h_t[:], in_=H_d.ap()[tile * 128:(tile + 1) * 128, :]), h_r, "ld", [rs["H_d"]], [h_r])
                          op("dve", "tensor_tensor", dict(out=sg_t[:], in0=V(iota2[:, 0, 0:1], [[0, NE], [1, CAPT * 128]]), in1=V(posm[:, tile, 0:1], [[1, NE], [0, CAPT * 128]]), op=ALU.is_equal), [iota2_r, posm_r], [sg_r])
                          op("dve", "tensor_tensor", dict(out=sg_t[:], in0=sg_t[:], in1=V(AFFS[:, tile, 0:1], [[1, NE], [0, CAPT * 128]]), op=MUL), [sg_r, AFFS_r], [sg_r])
                          for es in range(NES):
                              tr(sps[:, es, :], sg_t[:, es // CAPT, (es % CAPT) * 128:(es % CAPT + 1) * 128], [sg_r], [sps_r])
                          op("act", "activation", dict(out=sT_t[:], in_=sps[:], func=AF.Copy), [sps_r], [sT_r])
                          for half in range(2):
                              for es in range(NES):
                                  mm(yo_t[:, half, :], sT_t[:, es, :], Y_t[:, es, half * 512:(half + 1) * 512], es == 0, es == NES - 1, [sT_r, Y_r], [yo_r])
                          op("dve", "tensor_tensor", dict(out=o_t[:].rearrange("p (a b) -> p a b", a=2), in0=yo_t[:], in1=h_t[:].rearrange("p (a b) -> p a b", a=2), op=ADD), [yo_r, h_r], [o_r])
                          dst, dst_r = xdst_ap(l, tile)
                          dma("sp", dict(out=dst, in_=o_t[:]), o_r, "st", [o_r], [dst_r], merge=True)
          except StopBuild:
            break
        S.barrier()
        S.emit()
    return nc


def _host_consts(SEG):
    p = np.arange(128)
    cst = np.zeros((128, 640), np.float32)
    cst[:, 0:128] = (p[:, None] == p[None, :])
    cst[:, 128:256] = (p[:, None] <= p[None, :])
    cst[:, 256:384] = (p[:, None] >= p[None, :])
    cst[:, 384:512] = (p[:, None] <= p[None, :])
    cst[:, 512:640] = p[None, :].astype(np.float32)
    return cst


def _rope_table(positions):
    d = 64
    inv = (1.0 / (np.float32(10000.0) ** (np.arange(0, d, 2, dtype=np.float32) / np.float32(d)))).astype(np.float32)
    ang = positions.astype(np.float32)[:, None] * inv[None, :]
    c = np.cos(ang).astype(np.float32)
    s = np.sin(ang).astype(np.float32)
    return np.concatenate([c, c, -s, s], axis=1).astype(np.float32)


_NC_CACHE = {}


def run_cfg(inputs, L, NPS, SEG):
    key = (L, NPS, SEG)
    if key not in _NC_CACHE:
        _NC_CACHE[key] = build(dict(L=L, NPS=NPS, SEG=SEG))
    nc = _NC_CACHE[key]
    f = lambda k: np.ascontiguousarray(np.asarray(inputs[k], dtype=np.float32))
    x_prompt, x_sample = f("x_prompt"), f("x_sample")
    g4 = np.concatenate([f("diff_q_norm"), f("diff_k_norm"), f("swa_q_norm"), f("swa_k_norm")], axis=1)
    lam4 = np.concatenate([f("lambda_q1"), f("lambda_k1"), f("lambda_q2"), f("lambda_k2")], axis=1)
    cst = _host_consts(SEG)
    shared = dict(cst=cst, attn_norm=f("attn_norm"), w_in=f("w_in"), g4=np.ascontiguousarray(g4), lam4=np.ascontiguousarray(lam4),
                  subln=f("diff_subln"), sink=f("swa_sink"), w_out=f("w_out"), ffn_norm=f("ffn_norm"),
                  w_router=f("w_router"), w_gate=f("w_gate"), w_up=f("w_up"), w_down=f("w_down"))
    in_maps = []
    for i in range(8):
        seq, pos = i // 4, i % 4
        xp = x_prompt[i * NPS:(i + 1) * NPS].reshape(NPS * SEG, D)
        xs = x_sample[seq, pos * SEG:(pos + 1) * SEG]
        positions = np.concatenate([np.tile(np.arange(SEG), NPS), pos * SEG + np.arange(SEG)])
        cc = np.zeros((128, 16), np.float32)
        cc[:, 0] = 1.0 if pos > 0 else 0.0
        cc[:, 1] = 1.0 if pos < 3 else 0.0
        if pos > 0:
            cc[:, 2 + pos - 1] = 1.0
        if pos < 3:
            cc[:, 6 + pos + 1] = 1.0
        m = dict(shared)
        m.update(xp=np.ascontiguousarray(xp), xs=np.ascontiguousarray(xs), ropet=_rope_table(positions), cct=cc)
        in_maps.append(m)
    res = run_bass_kernel_spmd(nc, in_maps, core_ids=list(range(8)))
    y_prompt = np.zeros_like(x_prompt)
    y_sample = np.zeros_like(x_sample)
    for i in range(8):
        seq, pos = i // 4, i % 4
        r = res.results[i]
        y_prompt[i * NPS:(i + 1) * NPS] = np.asarray(r["yp"]).reshape(NPS, SEG, D)
        y_sample[seq, pos * SEG:(pos + 1) * SEG] = np.asarray(r["ys"])
    return y_prompt, y_sample


def kernel(**inputs):
    return run_cfg(inputs, 4, 4, 2048)
```
